# Optimizing a Trainium2 kernel written in Bass

```python
import jax, jax.numpy as jnp
from jax import lax
import numpy as np

D_MODEL = 1024
BATCH = 16
SEQ = 2048
DEPTH = 2

HEAD_DIM = 64
DIL_GROUPS = ((128, 1), (512, 4), (2048, 16))
DIL_HEADS_PER_GROUP = 4
DIL_HEADS = DIL_HEADS_PER_GROUP * len(DIL_GROUPS)
MOBA_HEADS = 4
MOBA_BLOCK = 256
MOBA_TOPK = 3
Q_CHUNK = 128
N_ATTN_HEADS = DIL_HEADS + MOBA_HEADS
DIL_WIDTH = DIL_HEADS * HEAD_DIM
MOBA_WIDTH = MOBA_HEADS * HEAD_DIM
DIL_OUT = DIL_HEADS_PER_GROUP * HEAD_DIM
PROJ_COLS = 3 * DIL_WIDTH + 3 * MOBA_WIDTH + 2 * D_MODEL
PROJ_SPLITS = (DIL_WIDTH, 2 * DIL_WIDTH, 3 * DIL_WIDTH,
               3 * DIL_WIDTH + MOBA_WIDTH, 3 * DIL_WIDTH + 2 * MOBA_WIDTH,
               3 * DIL_WIDTH + 3 * MOBA_WIDTH, 3 * DIL_WIDTH + 3 * MOBA_WIDTH + D_MODEL)
D_FF = 2816
CONV_WIDTH = 3
RMS_EPS = 1e-6
SCALE = HEAD_DIM ** -0.5

kernel_name = 'hybrid_dilated_moba_convffn_block'


def rms_norm(x, g):
    xf = x.astype(jnp.float32)
    y = xf * lax.rsqrt(jnp.mean(xf * xf, axis=-1, keepdims=True) + RMS_EPS)
    return (y * g.astype(jnp.float32)).astype(x.dtype)


def alibi_slopes():
    i = jnp.arange(1, N_ATTN_HEADS + 1, dtype=jnp.float32)
    return jnp.exp2(-8.0 * i / N_ATTN_HEADS)


def dilated_attention(q, k, v, window, dilation, slopes):
    B, S, H, hd = q.shape
    d = dilation
    steps = window // d
    blk = steps
    L = S // d
    Lp = -(-L // blk) * blk
    nblk = Lp // blk

    def to_blocks(a):
        a = a.reshape(B, L, d, H, hd)
        a = jnp.pad(a, ((0, 0), (0, Lp - L), (0, 0), (0, 0), (0, 0)))
        return a.reshape(B, nblk, blk, d, H, hd)

    def with_prev(a):
        prev = jnp.concatenate([jnp.zeros_like(a[:, :1]), a[:, :-1]], axis=1)
        return jnp.concatenate([prev, a], axis=2)

    qb = to_blocks(q)
    kc = with_prev(to_blocks(k))
    vc = with_prev(to_blocks(v))
    s = jnp.einsum('bnqrhd,bnkrhd->bnrhqk', qb, kc).astype(jnp.float32) * SCALE
    qi = jnp.arange(blk)
    ki = jnp.arange(2 * blk)
    delta = blk + qi[:, None] - ki[None, :]
    kstep = jnp.arange(nblk)[:, None, None] * blk - blk + ki[None, None, :]
    valid = (delta >= 0) & (delta <= steps) & (kstep >= 0)
    bias = -(slopes.astype(jnp.float32) * d)[:, None, None] * delta.astype(jnp.float32)
    s = jnp.where(valid[None, :, None, None], s + bias[None, None, None], -jnp.inf)
    lse = jax.nn.logsumexp(s, axis=-1)
    p = jnp.exp(s - lse[..., None]).astype(v.dtype)
    o = jnp.einsum('bnrhqk,bnkrhd->bnqrhd', p, vc)
    o = o.reshape(B, Lp, d, H, hd)[:, :L].reshape(B, S, H, hd)
    lse = lse.transpose(0, 1, 4, 2, 3).reshape(B, Lp, d, H)[:, :L].reshape(B, S, H)
    return o, lse


def moba_attention(q, k, v, slopes):
    B, S, H, hd = q.shape
    Sp = -(-S // MOBA_BLOCK) * MOBA_BLOCK
    nb = Sp // MOBA_BLOCK
    k_sel = min(MOBA_TOPK, nb - 1)
    pad = ((0, 0), (0, Sp - S), (0, 0), (0, 0))
    kblk = jnp.pad(k, pad).reshape(B, nb, MOBA_BLOCK, H, hd).transpose(0, 3, 1, 2, 4)
    vblk = jnp.pad(v, pad).reshape(B, nb, MOBA_BLOCK, H, hd).transpose(0, 3, 1, 2, 4)
    nq = S // Q_CHUNK
    slopes = slopes.astype(jnp.float32)
    q_chunks = q.reshape(B, nq, Q_CHUNK, H, hd).transpose(0, 1, 3, 2, 4).reshape(B * nq, H, Q_CHUNK, hd)
    b_ids = jnp.repeat(jnp.arange(B, dtype=jnp.int32), nq)
    c_ids = jnp.tile(jnp.arange(nq, dtype=jnp.int32), B)
    xs = (q_chunks, b_ids, c_ids)
    if k_sel > 0:
        pos = jnp.arange(S)
        own_blk = pos // MOBA_BLOCK
        kmean = jnp.mean(kblk.astype(jnp.float32), axis=3)
        gate = jnp.einsum('bshd,bhnd->bhsn', q.astype(jnp.float32), kmean)
        past = jnp.arange(nb)[None, :] < own_blk[:, None]
        gate = jnp.where(past[None, None], gate, -jnp.inf)
        _, gidx = lax.top_k(gate, k_sel)
        gvalid = gidx < own_blk[None, None, :, None]
        idx_chunks = gidx.reshape(B, H, nq, Q_CHUNK, k_sel).transpose(0, 2, 1, 3, 4).reshape(B * nq, H, Q_CHUNK, k_sel)
        val_chunks = gvalid.reshape(B, H, nq, Q_CHUNK, k_sel).transpose(0, 2, 1, 3, 4).reshape(B * nq, H, Q_CHUNK, k_sel)
        xs = xs + (idx_chunks, val_chunks)

    def body(args):
        qc, b, c = args[0], args[1], args[2]
        kb = kblk[b]
        vb = vblk[b]
        tq = c * Q_CHUNK + jnp.arange(Q_CHUNK)
        own = (c * Q_CHUNK) // MOBA_BLOCK
        k_own = lax.dynamic_index_in_dim(kb, own, axis=1, keepdims=False)
        v_own = lax.dynamic_index_in_dim(vb, own, axis=1, keepdims=False)
        tk_own = own * MOBA_BLOCK + jnp.arange(MOBA_BLOCK)
        dist_own = (tq[:, None] - tk_own[None, :]).astype(jnp.float32)
        s_own = jnp.einsum('hqd,hkd->hqk', qc, k_own).astype(jnp.float32) * SCALE - slopes[:, None, None] * dist_own
        s_own = jnp.where((dist_own >= 0)[None], s_own, -jnp.inf)
        if k_sel > 0:
            ic, vm = args[3], args[4]
            k_s = jax.vmap(lambda kh, ih: kh[ih])(kb, ic)
            v_s = jax.vmap(lambda vh, ih: vh[ih])(vb, ic)
            tk_s = ic[..., None] * MOBA_BLOCK + jnp.arange(MOBA_BLOCK)
            dist_s = (tq[None, :, None, None] - tk_s).astype(jnp.float32)
            s_s = jnp.einsum('hqd,hqknd->hqkn', qc, k_s).astype(jnp.float32) * SCALE - slopes[:, None, None, None] * dist_s
            s_s = jnp.where(vm[..., None], s_s, -jnp.inf).reshape(H, Q_CHUNK, k_sel * MOBA_BLOCK)
            p = jax.nn.softmax(jnp.concatenate([s_s, s_own], axis=-1), axis=-1).astype(v.dtype)
            n_s = k_sel * MOBA_BLOCK
            o = (jnp.einsum('hqn,hqnd->hqd', p[..., :n_s], v_s.reshape(H, Q_CHUNK, n_s, hd))
                 + jnp.einsum('hqk,hkd->hqd', p[..., n_s:], v_own))
        else:
            p = jax.nn.softmax(s_own, axis=-1).astype(v.dtype)
            o = jnp.einsum('hqk,hkd->hqd', p, v_own)
        return o.astype(v.dtype)

    out = lax.map(body, xs)
    return out.reshape(B, nq, H, Q_CHUNK, hd).transpose(0, 1, 3, 2, 4).reshape(B, S, H, hd)


def mixer_sublayer(x, g_pre, g_post, w_in, w_br_dil, w_br_moba, w_out):
    B, S, _ = x.shape
    h = rms_norm(x, g_pre)
    proj = h @ w_in
    qa, ka, va, qb, kb, vb, gate_a, gate_b = jnp.split(proj, PROJ_SPLITS, axis=-1)
    qa = qa.reshape(B, S, DIL_HEADS, HEAD_DIM)
    ka = ka.reshape(B, S, DIL_HEADS, HEAD_DIM)
    va = va.reshape(B, S, DIL_HEADS, HEAD_DIM)
    qb = qb.reshape(B, S, MOBA_HEADS, HEAD_DIM)
    kb = kb.reshape(B, S, MOBA_HEADS, HEAD_DIM)
    vb = vb.reshape(B, S, MOBA_HEADS, HEAD_DIM)
    slopes = alibi_slopes()
    outs, lses = [], []
    for g, (window, dilation) in enumerate(DIL_GROUPS):
        sl = slice(g * DIL_HEADS_PER_GROUP, (g + 1) * DIL_HEADS_PER_GROUP)
        o, lse = dilated_attention(qa[:, :, sl], ka[:, :, sl], va[:, :, sl], window, dilation, slopes[sl])
        outs.append(o)
        lses.append(lse)
    alpha = jax.nn.softmax(jnp.stack(lses, axis=0), axis=0)
    o_a = jnp.sum(alpha[..., None] * jnp.stack(outs, axis=0).astype(jnp.float32), axis=0).astype(x.dtype)
    o_b = moba_attention(qb, kb, vb, slopes[DIL_HEADS:])
    y_a = o_a.reshape(B, S, DIL_OUT) @ w_br_dil
    y_b = o_b.reshape(B, S, MOBA_WIDTH) @ w_br_moba
    merged = jax.nn.sigmoid(gate_a) * y_a + jax.nn.sigmoid(gate_b) * y_b
    return x + rms_norm(merged @ w_out, g_post)


def ffn_sublayer(x, g_pre, g_post, w_gate, w_up, conv_w, conv_b, w_down):
    h = rms_norm(x, g_pre)
    a = h @ w_gate
    a = lax.conv_general_dilated(a, conv_w.astype(a.dtype)[:, None, :], window_strides=(1,),
                                 padding=[(CONV_WIDTH - 1, 0)],
                                 dimension_numbers=('NWC', 'WIO', 'NWC'),
                                 feature_group_count=D_FF) + conv_b
    u = jax.nn.gelu(a, approximate=True) * (h @ w_up)
    return x + rms_norm(u @ w_down, g_post)


def setup_inputs(seed: int = 0) -> dict:
    key = jax.random.key(seed)
    ks = jax.random.split(key, 14)
    f32 = jnp.float32

    def nrm(k, shape, fan_in):
        return jax.random.normal(k, shape, f32) * (fan_in ** -0.5)

    def gain(k, n):
        return 1.0 + 0.05 * jax.random.normal(k, (DEPTH, n), f32)

    return {
        'x': jax.random.normal(ks[0], (BATCH, SEQ, D_MODEL), f32),
        'mix_norm_pre': gain(ks[1], D_MODEL),
        'mix_norm_post': gain(ks[2], D_MODEL),
        'w_in': nrm(ks[3], (DEPTH, D_MODEL, PROJ_COLS), D_MODEL),
        'w_branch_dil': nrm(ks[4], (DEPTH, DIL_OUT, D_MODEL), DIL_OUT),
        'w_branch_moba': nrm(ks[5], (DEPTH, MOBA_WIDTH, D_MODEL), MOBA_WIDTH),
        'w_out': nrm(ks[6], (DEPTH, D_MODEL, D_MODEL), D_MODEL),
        'ffn_norm_pre': gain(ks[7], D_MODEL),
        'ffn_norm_post': gain(ks[8], D_MODEL),
        'w_ffn_gate': nrm(ks[9], (DEPTH, D_MODEL, D_FF), D_MODEL),
        'w_ffn_up': nrm(ks[10], (DEPTH, D_MODEL, D_FF), D_MODEL),
        'ffn_conv_w': nrm(ks[11], (DEPTH, CONV_WIDTH, D_FF), CONV_WIDTH),
        'ffn_conv_b': 0.02 * jax.random.normal(ks[12], (DEPTH, D_FF), f32),
        'w_ffn_down': nrm(ks[13], (DEPTH, D_FF, D_MODEL), D_FF),
    }


def reference(x, mix_norm_pre, mix_norm_post, w_in, w_branch_dil, w_branch_moba, w_out,
              ffn_norm_pre, ffn_norm_post, w_ffn_gate, w_ffn_up, ffn_conv_w, ffn_conv_b, w_ffn_down):
    for l in range(DEPTH):
        x = mixer_sublayer(x, mix_norm_pre[l], mix_norm_post[l], w_in[l],
                           w_branch_dil[l], w_branch_moba[l], w_out[l])
        x = ffn_sublayer(x, ffn_norm_pre[l], ffn_norm_post[l], w_ffn_gate[l], w_ffn_up[l],
                         ffn_conv_w[l], ffn_conv_b[l], w_ffn_down[l])
    return x
```

```python
import contextlib
import numpy as np
import ml_dtypes
import concourse.bass as bass
import concourse.mybir as mybir
from concourse.bass_utils import run_bass_kernel_spmd

F32 = mybir.dt.float32
BF16 = mybir.dt.bfloat16
AF = mybir.ActivationFunctionType
ALU = mybir.AluOpType
AX = mybir.AxisListType

SEQ = 2048
D = 1024
DFF = 2816
NFC = 22
NCORES = 8
SCALE = 0.125
EPS = 1e-6
DILS = (1, 4, 16)
NEG = -30000.0


class SemObj:
    def __init__(self, nc, es, name):
        self.sem = es.enter_context(nc.semaphore(name))
        self.n = 0


class Q:
    def __init__(self, nc, es, eng, name):
        self.eng = eng
        self.so = SemObj(nc, es, "q_" + name)
        self.seen = {}
        self.name = name

    def wait(self, *toks):
        for t in toks:
            if t is None:
                continue
            if isinstance(t, list):
                self.wait(*t)
                continue
            so, v = t
            if self.seen.get(so, 0) >= v:
                continue
            self.eng.wait_ge(so.sem, v)
            self.seen[so] = v

    def do(self, inst):
        inst.then_inc(self.so.sem, 1)
        self.so.n += 1
        return (self.so, self.so.n)

    def now(self):
        return (self.so, self.so.n) if self.so.n > 0 else None


def build(n_seq=2, n_layers=2, do_mixer=True, do_ffn=True, mixer_stage=99, dump=False):
    nc = bass.Bass("TRN2", target_bir_lowering=False)

    def dt_in(name, shape, dt=F32):
        return nc.dram_tensor(name, list(shape), dt, kind="ExternalInput").ap()

    x_d = dt_in("x", [2, SEQ, D])
    win_d = dt_in("w_in_r", [2, 40, 128, 1024])
    wbrd_d = dt_in("w_brd_r", [2, 8, 128, 256])
    wbrm_d = dt_in("w_brm_r", [2, 8, 128, 256])
    wout_d = dt_in("w_out_r", [2, 8, 128, 1024])
    wgate_d = dt_in("w_gate_r", [2, NFC, 128, 1024])
    wup_d = dt_in("w_up_r", [2, NFC, 128, 1024])
    wdown_d = dt_in("w_down_r", [2, 8, 128, DFF])
    gains_d = dt_in("gains", [128, 2 * 4 * 8])
    convw_d = dt_in("convw", [128, 2 * NFC * 4])
    dmask_d = dt_in("dmask", [128, 12 * 256], BF16)
    tri_d = dt_in("tri", [128, 128], BF16)
    qaug_d = dt_in("qaug_t", [4, 64, SEQ], BF16)
    kaug_d = dt_in("kaug_t", [4, 64, SEQ], BF16)
    pastc_d = dt_in("pastc", [128, 3 * 128])
    ident_d = dt_in("ident", [128, 128])
    identb_d = dt_in("identb", [128, 128], BF16)
    y_d = nc.dram_tensor("y", [2, SEQ, D], F32, kind="ExternalOutput").ap()
    dbg_d = nc.dram_tensor("dbg", [128, 18432 + 8192], F32, kind="ExternalOutput").ap() if dump else None

    es = contextlib.ExitStack()
    with es:
        def sb(name, shape, dt):
            return es.enter_context(nc.sbuf_tensor("s_" + name, list(shape), dt))

        PE = Q(nc, es, nc.tensor, "pe")
        ACT = Q(nc, es, nc.scalar, "act")
        DVE = Q(nc, es, nc.vector, "dve")
        POOL = Q(nc, es, nc.gpsimd, "pool")
        SP = Q(nc, es, nc.sync, "sp")
        CQ = (PE, ACT, DVE)

        def dma(q, out, in_, so, **kw):
            q.eng.dma_start(out=out, in_=in_, **kw).then_inc(so.sem, 16)
            so.n += 16
            return (so, so.n)

        xT = sb("xT", [128, 8, SEQ], F32)
        hTb = sb("hTb", [128, 8 * SEQ], BF16)
        hT = hTb[:, :].rearrange("p (c t) -> p c t", c=8)
        ident = sb("ident", [128, 128], F32)
        identb = sb("identb", [128, 128], BF16)
        ones_bf = sb("ones_bf", [128, 128], BF16)
        gains = sb("gains", [128, 2, 4, 8], F32)
        convw = sb("convw", [128, 2, NFC, 4], F32)
        dmask = sb("dmask", [128, 12, 256], BF16)
        tri = sb("tri", [128, 128], BF16)
        pastc = sb("pastc", [128, 3, 16, 8], F32)
        epsb = sb("epsb", [128, 1], F32)
        halo = sb("halo", [128, NFC, 2], F32)
        ARENA_W = 18432
        arena = sb("arena", [128, ARENA_W], F32)
        NSLAB = 6
        slabs = sb("slabs", [128, NSLAB, 1024], BF16)
        wbr = sb("wbr", [128, 4, 256], BF16)
        psum = es.enter_context(nc.psum_tensor("psum", [128, 8 * 512], F32))
        ps = [psum[:, 512 * b:512 * (b + 1)] for b in range(8)]
        bank_free = [None] * 8

        def carve(off, nwords, dt=F32):
            assert off + nwords <= ARENA_W, (off, nwords)
            a = arena[:, off:off + nwords]
            if dt == BF16:
                a = a.bitcast(BF16)
            return a

        class BankSet:
            def __init__(self, ids):
                self.ids = list(ids)
                self.i = 0

            def get(self):
                b = self.ids[self.i % len(self.ids)]
                self.i += 1
                PE.wait(bank_free[b])
                bank_free[b] = None
                return b

        ALLB = BankSet(range(8))

        def barrier(extra=()):
            toks = [q.now() for q in CQ] + list(extra)
            for q in CQ:
                q.wait(*toks)
            return toks

        c_so = SemObj(nc, es, "const")
        dma(SP, ident[:, :], ident_d[:, :], c_so)
        dma(SP, identb[:, :], identb_d[:, :], c_so)
        dma(SP, gains[:, :, :, :].rearrange("p a b c -> p (a b c)"), gains_d[:, :], c_so)
        dma(SP, convw[:, :, :, :].rearrange("p a b c -> p (a b c)"), convw_d[:, :], c_so)
        dma(SP, dmask[:, :, :].rearrange("p a b -> p (a b)"), dmask_d[:, :], c_so)
        dma(SP, tri[:, :], tri_d[:, :], c_so)
        CT = dma(SP, pastc[:, :, :, :].rearrange("p a b c -> p (a b c)"), pastc_d[:, :], c_so)
        DVE.do(nc.vector.memset(ones_bf[:, :], 1.0 / 1024.0))
        DVE.do(nc.vector.memset(epsb[:, :], EPS))
        DVE.do(nc.vector.memset(halo[:, :, :], 0.0))
        barrier([CT])

        class Ring:
            def __init__(self, bufs, name):
                self.bufs = bufs
                self.so = [SemObj(nc, es, f"{name}{i}") for i in range(len(bufs))]
                self.free = [[] for _ in bufs]
                self.i = 0

            def load(self, src, n):
                s = self.i % len(self.bufs)
                self.i += 1
                POOL.wait(self.free[s])
                self.free[s] = []
                tok = dma(POOL, self.bufs[s][:, 0:n], src, self.so[s], max_dma_last_dim=4096)
                return self.bufs[s], tok, s

            def release(self, s, tok):
                self.free[s].append(tok)

        slab_ring = Ring([slabs[:, i, :] for i in range(NSLAB)], "slab")
        wbr_ring = Ring([wbr[:, i, :] for i in range(4)], "wbr")
        A_WDN = 11264
        wdn_ring = Ring([carve(A_WDN + i * 1408, 1408, BF16) for i in range(2)], "wdn")

        evac_i = [0]

        def evac_eng():
            evac_i[0] += 1
            return ACT if evac_i[0] % 2 else DVE

        def copy_on(q, out, in_, scale=None):
            if q is ACT:
                if scale is None:
                    return q.do(nc.scalar.copy(out=out, in_=in_))
                return q.do(nc.scalar.activation(out=out, in_=in_, func=AF.Copy, scale=float(scale)))
            if scale is None:
                return q.do(nc.vector.tensor_copy(out=out, in_=in_))
            return q.do(nc.vector.tensor_scalar(out=out, in0=in_, scalar1=float(scale), scalar2=None, op0=ALU.mult))

        def mm_group(out, pairs, waits=()):
            PE.wait(*waits)
            n = len(pairs)
            inst = None
            for i, (l_, r_) in enumerate(pairs):
                inst = nc.tensor.matmul(out, lhsT=l_, rhs=r_, start=(i == 0), stop=(i == n - 1))
            return PE.do(inst)

        xst = carve(0, 2048).rearrange("p (s f) -> p s f", s=2)
        xst_so = [SemObj(nc, es, f"xst{i}") for i in range(2)]
        yst_so = SemObj(nc, es, "yst")
        st = {}

        def load_x(s):
            barrier([st.get("store_tok")])
            SP.wait([q.now() for q in CQ])
            free = [[], []]
            for tt in range(16):
                sl = tt % 2
                SP.wait(free[sl])
                free[sl] = []
                tl = dma(SP, xst[:, sl, :], x_d[s, tt * 128:(tt + 1) * 128, :], xst_so[sl])
                tp = None
                for half in range(2):
                    b = ALLB.get()
                    PE.wait(tl)
                    inst = None
                    for j in range(4):
                        c = half * 4 + j
                        inst = nc.tensor.transpose(out=ps[b][:, j * 128:(j + 1) * 128],
                                                   in_=xst[:, sl, c * 128:(c + 1) * 128], identity=ident[:, :])
                    tp = PE.do(inst)
                    q = evac_eng()
                    q.wait(tp)
                    tc_ = copy_on(q, xT[:, half * 4:(half + 1) * 4, tt * 128:(tt + 1) * 128],
                                  ps[b].rearrange("p (c t) -> p c t", c=4))
                    bank_free[b] = [tc_]
                free[sl].append(tp)
            barrier()

        def store_x(s):
            barrier()
            free = [[], []]
            last = None
            for tt in range(16):
                sl = tt % 2
                tcs = []
                for half in range(2):
                    b = ALLB.get()
                    inst = None
                    for j in range(4):
                        c = half * 4 + j
                        inst = nc.tensor.transpose(out=ps[b][:, j * 128:(j + 1) * 128],
                                                   in_=xT[:, c, tt * 128:(tt + 1) * 128], identity=ident[:, :])
                    tp = PE.do(inst)
                    q = evac_eng()
                    q.wait(tp, free[sl])
                    tc_ = copy_on(q, xst[:, sl, half * 512:(half + 1) * 512], ps[b])
                    bank_free[b] = [tc_]
                    tcs.append(tc_)
                free[sl] = []
                SP.wait(tcs)
                td = dma(SP, y_d[s, tt * 128:(tt + 1) * 128, :], xst[:, sl, :], yst_so)
                free[sl].append(td)
                last = td
            st["store_tok"] = last
            for q in CQ:
                q.wait(last)

        dbg_so = SemObj(nc, es, "dbg")

        def do_dump():
            barrier()
            SP.wait([q.now() for q in CQ])
            dma(SP, dbg_d[:, 0:ARENA_W], arena[:, :], dbg_so)
            t = dma(SP, dbg_d[:, ARENA_W:ARENA_W + 8192], hTb[:, :].bitcast(F32), dbg_so)
            for q in CQ:
                q.wait(t)

        def rstd_from_sq(sq, waits, sd, rstd):
            b = ALLB.get()
            tp = mm_group(ps[b], [(ones_bf[:, :], sq[:, c, :]) for c in range(8)], waits)
            ACT.wait(tp)
            ta = ACT.do(nc.scalar.activation(out=sd, in_=ps[b], func=AF.Sqrt, bias=epsb[:, 0:1], scale=1.0))
            bank_free[b] = [ta]
            DVE.wait(ta)
            tr = DVE.do(nc.vector.reciprocal(out=rstd, in_=sd))
            return tr, tp

        def pre_norm(l, gi, dst, tg_list, scr_off):
            sqb = carve(scr_off, 4096, BF16).rearrange("p (s c t) -> p s c t", s=2, c=8)
            sdb = carve(scr_off + 4096, 1024).rearrange("p (s t) -> p s t", s=2)
            rsb = carve(scr_off + 5120, 1024).rearrange("p (s t) -> p s t", s=2)
            free = [[], []]
            for j, tg in enumerate(tg_list):
                sl = j % 2
                ACT.wait(free[sl])
                ts = ACT.do(nc.scalar.activation(out=sqb[:, sl, :, :], in_=xT[:, :, tg * 512:(tg + 1) * 512], func=AF.Square))
                DVE.wait(free[sl])
                free[sl] = []
                tr, tp = rstd_from_sq(sqb[:, sl], [ts], sdb[:, sl, :], rsb[:, sl, :])
                DVE.wait(tr)
                t = None
                for c in range(8):
                    t = DVE.do(nc.vector.scalar_tensor_tensor(
                        out=dst[:, c, j * 512:(j + 1) * 512], in0=xT[:, c, tg * 512:(tg + 1) * 512],
                        scalar=gains[:, l, gi, c:c + 1], in1=rsb[:, sl, :], op0=ALU.mult, op1=ALU.mult))
                free[sl] = [t, tp]

        def post_norm_update(l, gi, z, tg, waits, sq_off, sd_off):
            sqb = carve(sq_off, 2048, BF16).rearrange("p (c t) -> p c t", c=8)
            sd = carve(sd_off, 512)
            rs = carve(sd_off + 512, 512)
            fr = st.get("post_free")
            ACT.wait(waits, fr)
            ts = ACT.do(nc.scalar.activation(out=sqb, in_=z, func=AF.Square))
            DVE.wait(fr)
            tr, tp = rstd_from_sq(sqb, [ts], sd, rs)
            DVE.wait(tr, waits, ts)
            t1 = DVE.do(nc.vector.tensor_tensor(out=z, in0=z, in1=rs.unsqueeze(1).to_broadcast([128, 8, 512]), op=ALU.mult))
            DVE.wait(t1)
            t = None
            for c in range(8):
                t = DVE.do(nc.vector.scalar_tensor_tensor(
                    out=xT[:, c, tg * 512:(tg + 1) * 512], in0=z[:, c, :], scalar=gains[:, l, gi, c:c + 1],
                    in1=xT[:, c, tg * 512:(tg + 1) * 512], op0=ALU.mult, op1=ALU.add))
            st["post_free"] = [t, tp]
            return t

        A_V = 0
        A_QK = 6144
        A_P = 8192
        A_U = 9728
        A_RD = 13824
        A_OA = 14336
        A_OB = 2048
        A_MISC = 16384
        A_PN = 6144
        A_MT = 4096
        A_Z = 8192
        A_AB = 12288
        A_SQ = 0
        A_SD = 16384

        def mixer(l):
            Vb = [carve(A_V + i * 2048, 2048, BF16).rearrange("p (t c) -> p t c", t=16) for i in range(3)]
            QKb = [carve(A_QK + i * 1024, 1024, BF16) for i in range(2)]
            Pd = [carve(A_P + i * 128, 128, BF16) for i in range(12)]
            Pm = [carve(A_P + i * 256, 256, BF16) for i in range(6)]
            Ub = [carve(A_U + i * 2048, 2048) for i in range(2)]
            rD = carve(A_RD, 512)
            oaT = carve(A_OA, 2048, BF16).rearrange("p (c t) -> p c t", c=2)
            obT = carve(A_OB, 2048, BF16).rearrange("p (c t) -> p c t", c=2)
            m_ones = carve(A_MISC, 32, BF16)
            gm = carve(A_MISC + 64, 128).rearrange("p (t b) -> p t b", b=8)
            mx = carve(A_MISC + 192, 128).rearrange("p (t b) -> p t b", b=8)
            c1 = carve(A_MISC + 320, 128).rearrange("p (t b) -> p t b", b=8)
            selp = carve(A_MISC + 448, 64, BF16).rearrange("p (t b) -> p t b", b=8)
            km = carve(A_MISC + 512, 8)
            kmb = carve(A_MISC + 520, 4, BF16)

            barrier()
            pre_norm(l, 0, hT, [0, 1, 2, 3], A_PN)
            barrier()
            t_ones = DVE.do(nc.vector.memset(m_ones, 1.0))
            PE.wait(t_ones)

            def load_chunk(ci):
                return slab_ring.load(win_d[l, ci, :, :], 1024)

            def proj_fm(ci, consume):
                buf, tokw, s = load_chunk(ci)
                tp = None
                for tg in range(4):
                    b = ALLB.get()
                    tp = mm_group(ps[b], [(buf[:, kc * 128:(kc + 1) * 128], hT[:, kc, tg * 512:(tg + 1) * 512]) for kc in range(8)], [tokw])
                    tcs = consume(tg, b, tp)
                    bank_free[b] = list(tcs)
                slab_ring.release(s, tp)

            def v_proj(G, V, free_toks):
                c0 = 12 + 2 * G if G < 3 else 22
                b0, t0, s0 = load_chunk(c0)
                b1, t1, s1 = load_chunk(c0 + 1)
                d = DILS[G] if G < 3 else 1
                L = SEQ // d
                toks = []
                tp = None
                for tau in range(16):
                    r = (128 * tau) // L
                    i0 = (128 * tau) % L
                    start = i0 * d + r
                    b = ALLB.get()
                    PE.wait(t0, t1)
                    inst = None
                    for ci, bw in enumerate((b0, b1)):
                        for kc in range(8):
                            inst = nc.tensor.matmul(ps[b][:, ci * 128:(ci + 1) * 128],
                                                    lhsT=hT[:, kc, start:start + 127 * d + 1:d],
                                                    rhs=bw[:, kc * 128:(kc + 1) * 128], start=(kc == 0), stop=(kc == 7))
                    tp = PE.do(inst)
                    q = evac_eng()
                    q.wait(tp, free_toks)
                    tc_ = copy_on(q, V[:, tau, :], ps[b][:, 0:256])
                    bank_free[b] = [tc_]
                    toks.append(tc_)
                slab_ring.release(s0, tp)
                slab_ring.release(s1, tp)
                return toks

            def V_aug(V, tile, hd):
                v = V[:, tile, hd * 64:(hd + 1) * 64]
                return v, m_ones

            SB_ = BankSet([0, 1, 2, 3])
            UBA = BankSet([4, 6])
            UBB = BankSet([5, 7])

            def pv_mm(out, V, tile, hd, rhs, start, stop):
                nc.tensor.matmul(out[0:64, :], lhsT=V[:, tile, hd * 64:(hd + 1) * 64], rhs=rhs, start=start, stop=stop)
                return nc.tensor.matmul(out[64:128, :], lhsT=m_ones, rhs=rhs, start=start, stop=stop)

            def dil_attention(g, sp, Qs, Ks, qk_toks, V, v_toks, first, acc_free):
                d = DILS[g]
                L = SEQ // d
                nblk = L // 128
                SKEW = 2
                ptoks, pslot = {}, {}
                pfree = st.setdefault("pd_free", [[] for _ in range(12)])
                ucur = {}
                out_toks = []
                lastS = None
                for step in range(16 + SKEW):
                    if step < 16:
                        kt = step
                        nq = 256 if (kt % nblk) < nblk - 1 else 128
                        for h in range(2):
                            hp = 64 * h
                            bS = SB_.get()
                            PE.wait(qk_toks)
                            tS = PE.do(nc.tensor.matmul(ps[bS][:, 0:nq], lhsT=Ks[hp:hp + 64, kt * 128:(kt + 1) * 128],
                                                        rhs=Qs[hp:hp + 64, kt * 128:kt * 128 + nq], start=True, stop=True))
                            lastS = tS
                            sl = st.get("pd_i", 0) % 12
                            st["pd_i"] = st.get("pd_i", 0) + 1
                            ACT.wait(tS, pfree[sl])
                            pfree[sl] = []
                            tE = ACT.do(nc.scalar.activation(out=Pd[sl][:, 0:nq], in_=ps[bS][:, 0:nq], func=AF.Exp))
                            bank_free[bS] = [tE]
                            DVE.wait(tE)
                            head = g * 4 + 2 * sp + h
                            tM = DVE.do(nc.vector.tensor_tensor(out=Pd[sl][:, 0:nq], in0=Pd[sl][:, 0:nq],
                                                                in1=dmask[:, head, 0:nq], op=ALU.mult))
                            ptoks[(h, kt)] = tM
                            pslot[(h, kt)] = sl
                    if step >= SKEW:
                        qb = step - SKEW
                        n = qb % nblk
                        for h in range(2):
                            if qb % 4 == 0:
                                ucur[h] = (UBA if h == 0 else UBB).get()
                            ub = ucur[h]
                            col = (qb % 4) * 128
                            hd = 2 * sp + h
                            lst = []
                            if n > 0:
                                lst.append((qb - 1, pslot[(h, qb - 1)], 128))
                            lst.append((qb, pslot[(h, qb)], 0))
                            PE.wait(v_toks, ptoks[(h, qb)], ptoks.get((h, qb - 1)))
                            inst = None
                            for i, (ktile, sl, pc) in enumerate(lst):
                                inst = pv_mm(ps[ub][:, col:col + 128], V, ktile, hd, Pd[sl][:, pc:pc + 128],
                                             start=(i == 0), stop=(i == len(lst) - 1))
                            tU = PE.do(inst)
                            if n > 0:
                                pfree[pslot[(h, qb - 1)]].append(tU)
                            if n == nblk - 1:
                                pfree[pslot[(h, qb)]].append(tU)
                            if qb % 4 == 3:
                                m = qb // 4
                                U = Ub[h]
                                if d == 1:
                                    dst = U[:, m * 512:(m + 1) * 512]
                                    src = ps[ub]
                                elif d == 4:
                                    dst = U[:, m:SEQ:4]
                                    src = ps[ub]
                                else:
                                    dst = U.rearrange("p (i r) -> p r i", r=16)[:, 4 * m:4 * m + 4, :]
                                    src = ps[ub].rearrange("p (r i) -> p r i", r=4)
                                if first:
                                    ACT.wait(tU, acc_free)
                                    te = copy_on(ACT, dst, src)
                                else:
                                    DVE.wait(tU, st.get("u_last"))
                                    te = DVE.do(nc.vector.tensor_tensor(out=dst, in0=dst, in1=src, op=ALU.add))
                                bank_free[ub] = [te]
                                out_toks.append(te)
                st["u_last"] = out_toks
                return out_toks, lastS

            def normalize_sb(U, u_toks, slot, oT):
                toks = []
                rp = (slot % 2) * 64
                for tg in range(4):
                    DVE.wait(u_toks, st.get("rd_free"))
                    t1 = DVE.do(nc.vector.reciprocal(out=rD[0:64, :], in_=U[64:128, tg * 512:(tg + 1) * 512]))
                    DVE.wait(t1)
                    t2 = DVE.do(nc.vector.tensor_tensor(out=oT[rp:rp + 64, slot // 2, tg * 512:(tg + 1) * 512],
                                                        in0=U[0:64, tg * 512:(tg + 1) * 512], in1=rD[0:64, :], op=ALU.mult))
                    st["rd_free"] = [t2]
                    toks.append(t2)
                return toks

            vtoks = [v_proj(g, Vb[g], None) for g in range(3)]
            qk_free = None
            acc_free = None
            last_pv = None
            for sp in range(2):
                u_toks = None
                for g in range(3):
                    d = DILS[g]
                    qk_toks = []
                    for which in range(2):
                        ci = which * 6 + g * 2 + sp
                        dstb = QKb[which]

                        def consume(tg, b, tp, dstb=dstb, which=which, d=d):
                            q = evac_eng()
                            q.wait(tp, qk_free)
                            n_i = 512 // d
                            if d == 1:
                                o_ap = dstb[:, tg * 512:(tg + 1) * 512]
                                i_ap = ps[b]
                            else:
                                o_ap = dstb.rearrange("p (r i) -> p r i", r=d)[:, :, tg * n_i:(tg + 1) * n_i]
                                i_ap = ps[b].rearrange("p (i r) -> p r i", r=d)
                            t = copy_on(q, o_ap, i_ap, scale=(SCALE if which == 0 else None))
                            qk_toks.append(t)
                            return [t]
                        proj_fm(ci, consume)
                    u_toks, lastS = dil_attention(g, sp, QKb[0], QKb[1], qk_toks, Vb[g], vtoks[g], g == 0, acc_free)
                    qk_free = [lastS]
                    last_pv = PE.now()
                nt = []
                for h in range(2):
                    nt += normalize_sb(Ub[h], u_toks, 2 * sp + h, oaT)
                acc_free = nt
            if mixer_stage < 2:
                return

            AUG = [QKb[0], QKb[1], carve(A_V + 4096, 1024, BF16), carve(A_V + 5120, 1024, BF16)]
            barrier()
            vm_toks = v_proj(3, Vb[0], None)
            if "aug_so" not in st:
                st["aug_so"] = [SemObj(nc, es, f"aug{i}") for i in range(4)]
            aug_so = st["aug_so"]
            for hpair in range(2):
                QA, KA, QB, KB = AUG
                heads = (2 * hpair, 2 * hpair + 1)
                SP.wait([q.now() for q in CQ])
                tt_ = [dma(SP, QA[64:128, :], qaug_d[heads[0], :, :], aug_so[0]),
                       dma(SP, KA[64:128, :], kaug_d[heads[0], :, :], aug_so[1]),
                       dma(SP, QB[0:64, :], qaug_d[heads[1], :, :], aug_so[2]),
                       dma(SP, KB[0:64, :], kaug_d[heads[1], :, :], aug_so[3])]
                qk_toks = []
                for which in range(2):
                    ci = 18 + 2 * which + hpair
                    dA, dB = (QA, QB) if which == 0 else (KA, KB)

                    def consume(tg, b, tp, dA=dA, dB=dB, which=which):
                        sc = SCALE if which == 0 else None
                        ACT.wait(tp)
                        ta = copy_on(ACT, dA[0:64, tg * 512:(tg + 1) * 512], ps[b][0:64, :], scale=sc)
                        DVE.wait(tp)
                        tb = copy_on(DVE, dB[64:128, tg * 512:(tg + 1) * 512], ps[b][64:128, :], scale=sc)
                        qk_toks.extend([ta, tb])
                        return [ta, tb]
                    proj_fm(ci, consume)
                for h in range(2):
                    Qx, Kx = (QA, KA) if h == 0 else (QB, KB)
                    r0 = 0 if h == 0 else 64
                    e0 = 64 if h == 0 else 0
                    DVE.wait(qk_toks, st.get("km_free"))
                    tk1 = DVE.do(nc.vector.tensor_reduce(out=km[r0:r0 + 64, :], in_=Kx[r0:r0 + 64, :].rearrange("p (b k) -> p b k", b=8),
                                                         axis=AX.X, op=ALU.add))
                    DVE.wait(tk1)
                    tk2 = DVE.do(nc.vector.tensor_scalar(out=kmb[r0:r0 + 64, :], in0=km[r0:r0 + 64, :], scalar1=1.0 / 256.0, scalar2=None, op0=ALU.mult))
                    bg = SB_.get()
                    PE.wait(tk2, qk_toks)
                    inst = None
                    for tau in range(16):
                        inst = nc.tensor.matmul(ps[bg][:, tau * 8:(tau + 1) * 8], lhsT=Qx[r0:r0 + 64, tau * 128:(tau + 1) * 128],
                                                rhs=kmb[r0:r0 + 64, :], start=True, stop=True)
                    tg_ = PE.do(inst)
                    st["km_free"] = [tg_]
                    DVE.wait(tg_, st.get("gm_free"))
                    t1 = DVE.do(nc.vector.tensor_tensor(out=gm, in0=ps[bg][:, 0:128].rearrange("p (t b) -> p t b", b=8),
                                                        in1=pastc[:, 0, :, :], op=ALU.add))
                    bank_free[bg] = [t1]
                    DVE.wait(t1)
                    t2 = None
                    for tau in range(16):
                        t2 = DVE.do(nc.vector.max(out=mx[:, tau, :], in_=gm[:, tau, :]))
                    DVE.wait(t2)
                    t3 = DVE.do(nc.vector.tensor_tensor(out=c1, in0=gm, in1=mx[:, :, 2:3].to_broadcast([128, 16, 8]), op=ALU.is_ge))
                    DVE.wait(t3)
                    t4 = DVE.do(nc.vector.tensor_tensor(out=c1, in0=c1, in1=pastc[:, 1, :, :], op=ALU.mult))
                    DVE.wait(t4)
                    t5 = DVE.do(nc.vector.tensor_tensor(out=c1, in0=c1, in1=pastc[:, 2, :, :], op=ALU.add))
                    DVE.wait(t5)
                    t6 = DVE.do(nc.vector.tensor_scalar(out=selp, in0=c1, scalar1=-1.0, scalar2=-NEG, op0=ALU.add, op1=ALU.mult))
                    b1 = SB_.get()
                    b2 = SB_.get()
                    PE.wait(t6)
                    inst = None
                    for tau in range(16):
                        bb = b1 if tau < 8 else b2
                        pso = ps[bb].bitcast(BF16)
                        inst = nc.tensor.transpose(out=pso[0:8, (tau % 8) * 128:(tau % 8 + 1) * 128], in_=selp[:, tau, :], identity=identb[:, :])
                    tT = PE.do(inst)
                    st["gm_free"] = [tT]
                    ACT.wait(tT, tt_)
                    ta = ACT.do(nc.scalar.copy(out=Qx[e0:e0 + 8, 0:1024], in_=ps[b1].bitcast(BF16)[0:8, :]))
                    DVE.wait(tT, tt_)
                    tb = DVE.do(nc.vector.tensor_copy(out=Qx[e0:e0 + 8, 1024:2048], in_=ps[b2].bitcast(BF16)[0:8, :]))
                    bank_free[b1] = [ta]
                    bank_free[b2] = [tb]
                    qk_toks.extend([ta, tb])
                units = [(qg, kt, h) for qg in range(4) for kt in range(4 * qg + 4) for h in range(2)]
                SKEW = 4
                info = {}
                pfree = st.setdefault("pm_free", [[] for _ in range(6)])
                ucur = {}
                for i in range(len(units) + SKEW):
                    if i < len(units):
                        qg, kt, h = units[i]
                        Qx, Kx = (QA, KA) if h == 0 else (QB, KB)
                        c0 = max(0, kt - 4 * qg) * 128
                        nq = 512 - c0
                        bS = SB_.get()
                        PE.wait(qk_toks, tt_)
                        tS = PE.do(nc.tensor.matmul(ps[bS][:, 0:nq], lhsT=Kx[:, kt * 128:(kt + 1) * 128],
                                                    rhs=Qx[:, qg * 512 + c0:(qg + 1) * 512], start=True, stop=True))
                        sl = st.get("pm_i", 0) % 6
                        st["pm_i"] = st.get("pm_i", 0) + 1
                        ACT.wait(tS, pfree[sl])
                        pfree[sl] = []
                        tE = ACT.do(nc.scalar.activation(out=Pm[sl][:, 0:nq], in_=ps[bS][:, 0:nq], func=AF.Exp))
                        bank_free[bS] = [tE]
                        if kt >= 4 * qg:
                            DVE.wait(tE)
                            tE = DVE.do(nc.vector.tensor_tensor(out=Pm[sl][:, 0:128], in0=Pm[sl][:, 0:128], in1=tri[:, :], op=ALU.mult))
                        info[i] = (tE, sl, c0, nq)
                    if i >= SKEW:
                        j = i - SKEW
                        qg, kt, h = units[j]
                        tE, sl, c0, nq = info.pop(j)
                        if kt == 0:
                            ucur[h] = (UBA if h == 0 else UBB).get()
                        ub = ucur[h]
                        hd = 2 * hpair + h
                        PE.wait(tE, vm_toks)
                        last = (kt == 4 * qg + 3)
                        inst = pv_mm(ps[ub][:, c0:512], Vb[0], kt, hd, Pm[sl][:, 0:nq], start=(kt == 0), stop=last)
                        tU = PE.do(inst)
                        pfree[sl].append(tU)
                        if last:
                            DVE.wait(tU, st.get("rd_free"))
                            t1 = DVE.do(nc.vector.reciprocal(out=rD[0:64, :], in_=ps[ub][64:128, :]))
                            DVE.wait(t1)
                            rp = (hd % 2) * 64
                            t2 = DVE.do(nc.vector.tensor_tensor(out=obT[rp:rp + 64, hd // 2, qg * 512:(qg + 1) * 512],
                                                                in0=ps[ub][0:64, :], in1=rD[0:64, :], op=ALU.mult))
                            st["rd_free"] = [t2]
                            bank_free[ub] = [t2]
                barrier()
            if dump == "attn":
                do_dump()
                return
            if mixer_stage < 3:
                return

            mT = carve(A_MT, 4096, BF16).rearrange("p (c t) -> p c t", c=8)
            z = carve(A_Z, 4096).rearrange("p (c t) -> p c t", c=8)
            AB = [carve(A_AB + i * 512, 512) for i in range(4)]
            barrier()
            ab_free = [[], [], [], []]
            for hf in range(2):
                mt_toks = []
                for oc in range(8):
                    bga, tga, sga = load_chunk(24 + oc)
                    bgb, tgb, sgb = load_chunk(32 + oc)
                    bwa, twa, swa = wbr_ring.load(wbrd_d[l, oc, :, :], 256)
                    bwm, twm, swm = wbr_ring.load(wbrm_d[l, oc, :, :], 256)
                    tp = None
                    for tgi in range(2):
                        tg = 2 * hf + tgi
                        tsl = slice(tg * 512, (tg + 1) * 512)
                        b_ga = ALLB.get()
                        t_ga = mm_group(ps[b_ga], [(bga[:, kc * 128:(kc + 1) * 128], hT[:, kc, tsl]) for kc in range(8)], [tga])
                        b_gb = ALLB.get()
                        t_gb = mm_group(ps[b_gb], [(bgb[:, kc * 128:(kc + 1) * 128], hT[:, kc, tsl]) for kc in range(8)], [tgb])
                        b_ya = ALLB.get()
                        t_ya = mm_group(ps[b_ya], [(bwa[:, kc * 128:(kc + 1) * 128], oaT[:, kc, tsl]) for kc in range(2)], [twa])
                        b_yb = ALLB.get()
                        t_yb = mm_group(ps[b_yb], [(bwm[:, kc * 128:(kc + 1) * 128], obT[:, kc, tsl]) for kc in range(2)], [twm])
                        tp = t_yb
                        ia = (tgi % 2) * 2
                        A_, B_ = AB[ia], AB[ia + 1]
                        ACT.wait(t_ga, ab_free[ia])
                        ab_free[ia] = []
                        s1 = ACT.do(nc.scalar.activation(out=A_, in_=ps[b_ga], func=AF.Sigmoid))
                        bank_free[b_ga] = [s1]
                        ACT.wait(t_gb, ab_free[ia + 1])
                        ab_free[ia + 1] = []
                        s2 = ACT.do(nc.scalar.activation(out=B_, in_=ps[b_gb], func=AF.Sigmoid))
                        bank_free[b_gb] = [s2]
                        DVE.wait(s1, t_ya)
                        m1 = DVE.do(nc.vector.tensor_tensor(out=A_, in0=A_, in1=ps[b_ya], op=ALU.mult))
                        bank_free[b_ya] = [m1]
                        DVE.wait(s2, t_yb)
                        m2 = DVE.do(nc.vector.tensor_tensor(out=B_, in0=B_, in1=ps[b_yb], op=ALU.mult))
                        bank_free[b_yb] = [m2]
                        DVE.wait(m1, m2, st.get("mt_free"))
                        m3 = DVE.do(nc.vector.tensor_tensor(out=mT[:, oc, tgi * 512:(tgi + 1) * 512], in0=A_, in1=B_, op=ALU.add))
                        ab_free[ia] = [m3]
                        ab_free[ia + 1] = [m3]
                        mt_toks.append(m3)
                    slab_ring.release(sga, tp)
                    slab_ring.release(sgb, tp)
                    wbr_ring.release(swa, tp)
                    wbr_ring.release(swm, tp)
                for tgi in range(2):
                    tg = 2 * hf + tgi
                    zt = []
                    for oc in range(8):
                        bw, tw, sw = slab_ring.load(wout_d[l, oc, :, :], 1024)
                        b = ALLB.get()
                        tp = mm_group(ps[b], [(bw[:, kc * 128:(kc + 1) * 128], mT[:, kc, tgi * 512:(tgi + 1) * 512]) for kc in range(8)], [tw, mt_toks])
                        slab_ring.release(sw, tp)
                        q = evac_eng()
                        q.wait(tp, st.get("post_free"))
                        tc_ = copy_on(q, z[:, oc, :], ps[b])
                        bank_free[b] = [tc_]
                        zt.append(tc_)
                    post_norm_update(l, 1, z, tg, zt, A_SQ, A_SD)
                st["mt_free"] = [PE.now()]
            barrier()

        F_UT = 0
        F_A = 14080
        F_T1 = 15112
        F_PN = 0
        F_SQ = 0
        F_SD = 16384

        def ffn(l):
            h2 = hTb[:, 0:8192].rearrange("p (c t) -> p c t", c=8)
            zf = hTb[:, :].bitcast(F32).rearrange("p (c t) -> p c t", c=8)
            uT = carve(F_UT, 11264, BF16).rearrange("p (c t) -> p c t", c=NFC)
            a_sb = [carve(F_A + i * 516, 516) for i in range(2)]
            t1b = [carve(F_T1 + i * 512, 512) for i in range(2)]
            for hf in range(2):
                barrier()
                pre_norm(l, 2, h2, [2 * hf, 2 * hf + 1], F_PN)
                barrier()
                cfree = [[], []]
                ci_ = 0
                for fc in range(NFC):
                    bg, tg_w, sg = slab_ring.load(wgate_d[l, fc, :, :], 1024)
                    bu, tu_w, su = slab_ring.load(wup_d[l, fc, :, :], 1024)
                    tp = None
                    for tgi in range(2):
                        tsl = slice(tgi * 512, (tgi + 1) * 512)
                        b_a = ALLB.get()
                        t_a = mm_group(ps[b_a], [(bg[:, kc * 128:(kc + 1) * 128], h2[:, kc, tsl]) for kc in range(8)], [tg_w])
                        b_u = ALLB.get()
                        t_u = mm_group(ps[b_u], [(bu[:, kc * 128:(kc + 1) * 128], h2[:, kc, tsl]) for kc in range(8)], [tu_w])
                        tp = t_u
                        sl = ci_ % 2
                        ci_ += 1
                        a_ = a_sb[sl]
                        t1_ = t1b[sl]
                        ACT.wait(t_a, cfree[sl], st.get("halo_tok"))
                        cfree[sl] = []
                        th = ACT.do(nc.scalar.copy(out=a_[:, 0:2], in_=halo[:, fc, :]))
                        tc_ = ACT.do(nc.scalar.copy(out=a_[:, 2:514], in_=ps[b_a]))
                        ACT.wait(tc_)
                        if not (hf == 1 and tgi == 1):
                            th2 = ACT.do(nc.scalar.copy(out=halo[:, fc, :], in_=a_[:, 512:514]))
                        else:
                            th2 = ACT.do(nc.scalar.copy(out=halo[:, fc, :], in_=a_[:, 0:2]))
                        st["halo_tok"] = [th2]
                        tt1 = ACT.do(nc.scalar.activation(out=t1_, in_=ps[b_a], func=AF.Identity,
                                                          bias=convw[:, l, fc, 3:4], scale=convw[:, l, fc, 2:3]))
                        bank_free[b_a] = [tt1]
                        DVE.wait(tt1, tc_, th)
                        d1 = DVE.do(nc.vector.scalar_tensor_tensor(out=t1_, in0=a_[:, 1:513], scalar=convw[:, l, fc, 1:2],
                                                                   in1=t1_, op0=ALU.mult, op1=ALU.add))
                        DVE.wait(d1)
                        d2 = DVE.do(nc.vector.scalar_tensor_tensor(out=t1_, in0=a_[:, 0:512], scalar=convw[:, l, fc, 0:1],
                                                                   in1=t1_, op0=ALU.mult, op1=ALU.add))
                        ACT.wait(d2)
                        g1 = ACT.do(nc.scalar.activation(out=t1_, in_=t1_, func=AF.Gelu_apprx_tanh))
                        DVE.wait(g1, t_u)
                        u1 = DVE.do(nc.vector.tensor_tensor(out=uT[:, fc, tsl], in0=t1_, in1=ps[b_u], op=ALU.mult))
                        bank_free[b_u] = [u1]
                        cfree[sl] = [u1]
                    slab_ring.release(sg, tp)
                    slab_ring.release(su, tp)
                if hf == 1:
                    DVE.wait(st.get("halo_tok"))
                    tz = DVE.do(nc.vector.memset(halo[:, :, :], 0.0))
                    st["halo_tok"] = [tz]
                barrier()
                for oc in range(8):
                    bw, tw, sw = wdn_ring.load(wdown_d[l, oc, :, :], DFF)
                    tp = None
                    for tgi in range(2):
                        b = ALLB.get()
                        tp = mm_group(ps[b], [(bw[:, kc * 128:(kc + 1) * 128], uT[:, kc, tgi * 512:(tgi + 1) * 512]) for kc in range(NFC)], [tw])
                        q = evac_eng()
                        q.wait(tp)
                        tc_ = copy_on(q, zf[:, oc, tgi * 512:(tgi + 1) * 512], ps[b])
                        bank_free[b] = [tc_]
                    wdn_ring.release(sw, tp)
                barrier()
                for tgi in range(2):
                    post_norm_update(l, 3, zf[:, :, tgi * 512:(tgi + 1) * 512], 2 * hf + tgi, [], F_SQ, F_SD)
                barrier()

        for s in range(n_seq):
            load_x(s)
            for l in range(n_layers):
                if do_mixer:
                    mixer(l)
                if do_ffn:
                    ffn(l)
            store_x(s)
        for q in CQ + (SP,):
            q.wait(st["store_tok"])
    return nc


def _slopes():
    i = np.arange(1, 17, dtype=np.float32)
    return np.exp2(-8.0 * i / 16).astype(np.float32)


def _chunk_layout(w, kdim):
    nk = kdim // 128
    noc = w.shape[1] // 128
    return np.ascontiguousarray(w.reshape(nk, 128, noc, 128).transpose(2, 1, 0, 3).reshape(noc, 128, nk * 128))


def _bf16_hi_lo(a):
    hi = a.astype(ml_dtypes.bfloat16)
    lo = (a - hi.astype(np.float32)).astype(ml_dtypes.bfloat16)
    return hi, lo


def _constants():
    sl = _slopes()
    bf = ml_dtypes.bfloat16
    k = np.arange(128)[:, None].astype(np.float32)
    q = np.arange(256)[None, :].astype(np.float32)
    delta = q - k
    valid = (delta >= 0) & (delta <= 128)
    dmask = np.zeros((128, 12, 256), np.float32)
    for g in range(3):
        for j in range(4):
            h = g * 4 + j
            dmask[:, h, :] = np.where(valid, np.exp(-sl[h] * DILS[g] * np.where(valid, delta, 0.0)), 0.0)
    tri = (np.arange(128)[None, :] >= np.arange(128)[:, None]).astype(np.float32)
    t = np.arange(SEQ)
    qaug = np.zeros((4, 64, SEQ), np.float32)
    kaug = np.zeros((4, 64, SEQ), np.float32)
    qaug_b = np.zeros((4, 64, SEQ), bf)
    kaug_b = np.zeros((4, 64, SEQ), bf)
    for hm in range(4):
        m = np.float32(sl[12 + hm])
        for b in range(8):
            ind = (t // 256 == b).astype(np.float32)
            A = (-m * (t - 256 * b)).astype(np.float32)
            hi, lo = _bf16_hi_lo(A)
            for base in (0, 8, 16):
                kaug_b[hm, base + b] = ind.astype(bf)
            qaug_b[hm, 8 + b] = hi
            qaug_b[hm, 16 + b] = lo
        kp = (m * (t % 256)).astype(np.float32)
        hi, lo = _bf16_hi_lo(kp)
        kaug_b[hm, 24] = hi
        kaug_b[hm, 25] = lo
        qaug_b[hm, 24] = np.ones(SEQ, bf)
        qaug_b[hm, 25] = np.ones(SEQ, bf)
    pastc = np.zeros((128, 3, 16, 8), np.float32)
    for tau in range(16):
        for b in range(8):
            pastc[:, 0, tau, b] = 0.0 if b < tau // 2 else -1e30
            pastc[:, 1, tau, b] = 1.0 if b < tau // 2 else 0.0
            pastc[:, 2, tau, b] = 1.0 if b == tau // 2 else 0.0
    return {
        "dmask": np.ascontiguousarray(dmask.reshape(128, 12 * 256)).astype(bf),
        "tri": tri.astype(bf),
        "qaug_t": qaug_b, "kaug_t": kaug_b,
        "pastc": np.ascontiguousarray(pastc.reshape(128, 384)),
        "ident": np.eye(128, dtype=np.float32),
        "identb": np.eye(128, dtype=np.float32).astype(bf),
    }


def prep_shared(inp, n_layers=2):
    f = lambda a: np.asarray(a, dtype=np.float32)
    sh = {}
    sh["w_in_r"] = np.stack([_chunk_layout(f(inp["w_in"][l]), 1024) for l in range(2)])
    sh["w_brd_r"] = np.stack([_chunk_layout(f(inp["w_branch_dil"][l]), 256) for l in range(2)])
    sh["w_brm_r"] = np.stack([_chunk_layout(f(inp["w_branch_moba"][l]), 256) for l in range(2)])
    sh["w_out_r"] = np.stack([_chunk_layout(f(inp["w_out"][l]), 1024) for l in range(2)])
    sh["w_gate_r"] = np.stack([_chunk_layout(f(inp["w_ffn_gate"][l]), 1024) for l in range(2)])
    sh["w_up_r"] = np.stack([_chunk_layout(f(inp["w_ffn_up"][l]), 1024) for l in range(2)])
    sh["w_down_r"] = np.stack([_chunk_layout(f(inp["w_ffn_down"][l]), DFF) for l in range(2)])
    g = np.stack([f(inp["mix_norm_pre"]), f(inp["mix_norm_post"]), f(inp["ffn_norm_pre"]), f(inp["ffn_norm_post"])], axis=1)
    sh["gains"] = np.ascontiguousarray(g.reshape(2, 4, 8, 128).transpose(3, 0, 1, 2).reshape(128, 64))
    cw = np.concatenate([f(inp["ffn_conv_w"]), f(inp["ffn_conv_b"])[:, None, :]], axis=1)
    sh["convw"] = np.ascontiguousarray(cw.reshape(2, 4, NFC, 128).transpose(3, 0, 2, 1).reshape(128, 2 * NFC * 4))
    sh.update(_constants())
    return sh


_NC_CACHE = {}


def kernel(**inputs):
    x = np.asarray(inputs["x"], dtype=np.float32)
    sh = prep_shared(inputs)
    if "nc" not in _NC_CACHE:
        _NC_CACHE["nc"] = build()
    nc = _NC_CACHE["nc"]
    in_maps = []
    for c in range(NCORES):
        m = dict(sh)
        m["x"] = np.ascontiguousarray(x[2 * c:2 * c + 2])
        in_maps.append(m)
    res = run_bass_kernel_spmd(nc, in_maps, core_ids=list(range(NCORES)))
    out = np.concatenate([np.asarray(r["y"], dtype=np.float32) for r in res.results], axis=0)
    return out
```

```python
import contextlib
import numpy as np
import ml_dtypes
import concourse.bass as bass
import concourse.mybir as mybir
from concourse.bass_utils import run_bass_kernel_spmd

F32 = mybir.dt.float32
BF16 = mybir.dt.bfloat16
AF = mybir.ActivationFunctionType
ALU = mybir.AluOpType
AX = mybir.AxisListType

SEQ = 2048
D = 1024
DFF = 2816
NFC = 22
NCORES = 8
SCALE = 0.125
EPS = 1e-6
DILS = (1, 4, 16)
NEG = -30000.0


class SemObj:
    def __init__(self, nc, es, name):
        self.sem = es.enter_context(nc.semaphore(name))
        self.n = 0


class Q:
    def __init__(self, nc, es, eng, name, attach=False):
        self.eng = eng
        self.so = SemObj(nc, es, "q_" + name)
        self.seen = {}
        self.name = name
        self.attach = attach
        self.pending = None

    def _collect(self, toks, acc):
        for t in toks:
            if t is None:
                continue
            if isinstance(t, (list, tuple)) and not (len(t) == 2 and isinstance(t[0], SemObj)):
                self._collect(t, acc)
                continue
            so, v = t
            if acc.get(so, 0) < v:
                acc[so] = v

    def wait(self, *toks):
        acc = {}
        self._collect(toks, acc)
        for so, v in acc.items():
            if self.seen.get(so, 0) >= v:
                continue
            self.seen[so] = v
            if self.pending is not None:
                self.eng.wait_ge(self.pending[0].sem, self.pending[1])
                self.pending = None
            if self.attach:
                self.pending = (so, v)
            else:
                self.eng.wait_ge(so.sem, v)

    def flush(self):
        if self.pending is not None:
            self.eng.wait_ge(self.pending[0].sem, self.pending[1])
            self.pending = None

    def do(self, inst):
        if self.pending is not None:
            inst._wait_ge(self.pending[0].sem, self.pending[1])
            self.pending = None
        inst.then_inc(self.so.sem, 1)
        self.so.n += 1
        return (self.so, self.so.n)

    def now(self):
        return (self.so, self.so.n) if self.so.n > 0 else None


def build(n_seq=2, n_layers=2, do_mixer=True, do_ffn=True, mixer_stage=99, dump=False):
    nc = bass.Bass("TRN2", target_bir_lowering=False)

    def dt_in(name, shape, dt=F32):
        return nc.dram_tensor(name, list(shape), dt, kind="ExternalInput").ap()

    x_d = dt_in("x", [2, SEQ, D])
    win_d = dt_in("w_in_r", [2, 40, 128, 1024])
    wbrd_d = dt_in("w_brd_r", [2, 8, 128, 256])
    wbrm_d = dt_in("w_brm_r", [2, 8, 128, 256])
    wout_d = dt_in("w_out_r", [2, 8, 128, 1024])
    wgate_d = dt_in("w_gate_r", [2, NFC, 128, 1024])
    wup_d = dt_in("w_up_r", [2, NFC, 128, 1024])
    wdown_d = dt_in("w_down_r", [2, 8, 128, DFF])
    gains_d = dt_in("gains", [128, 2 * 4 * 8])
    convw_d = dt_in("convw", [128, 2 * NFC * 4])
    dmask_d = dt_in("dmask", [128, 12 * 256], BF16)
    tri_d = dt_in("tri", [128, 128], BF16)
    qaug_d = dt_in("qaug_t", [4, 64, SEQ], BF16)
    kaug_d = dt_in("kaug_t", [4, 64, SEQ], BF16)
    pastc_d = dt_in("pastc", [128, 3 * 128])
    ident_d = dt_in("ident", [128, 128])
    identb_d = dt_in("identb", [128, 128], BF16)
    y_d = nc.dram_tensor("y", [2, SEQ, D], F32, kind="ExternalOutput").ap()
    dbg_d = nc.dram_tensor("dbg", [128, 21504 + 8192], F32, kind="ExternalOutput").ap() if dump else None

    es = contextlib.ExitStack()
    with es:
        def sb(name, shape, dt):
            return es.enter_context(nc.sbuf_tensor("s_" + name, list(shape), dt))

        PE = Q(nc, es, nc.tensor, "pe")
        ACT = Q(nc, es, nc.scalar, "act", attach=True)
        DVE = Q(nc, es, nc.vector, "dve", attach=True)
        POOL = Q(nc, es, nc.gpsimd, "pool")
        SP = Q(nc, es, nc.sync, "sp")
        CQ = (PE, ACT, DVE)

        def dma(q, out, in_, so, **kw):
            q.eng.dma_start(out=out, in_=in_, **kw).then_inc(so.sem, 16)
            so.n += 16
            return (so, so.n)

        xT = sb("xT", [128, 8, SEQ], F32)
        hTb = sb("hTb", [128, 8 * SEQ], BF16)
        hT = hTb[:, :].rearrange("p (c t) -> p c t", c=8)
        ident = sb("ident", [128, 128], F32)
        identb = sb("identb", [128, 128], BF16)
        ones_bf = sb("ones_bf", [128, 128], BF16)
        gains = sb("gains", [128, 2, 4, 8], F32)
        convw = sb("convw", [128, 2, NFC, 4], F32)
        dmask = sb("dmask", [128, 12, 256], BF16)
        tri = sb("tri", [128, 128], BF16)
        pastc = sb("pastc", [128, 3, 16, 8], F32)
        epsb = sb("epsb", [128, 1], F32)
        halo = sb("halo", [128, NFC, 2], F32)
        ARENA_W = 21504
        arena = sb("arena", [128, ARENA_W], F32)
        NSLAB = 6
        slabs = sb("slabs", [128, NSLAB, 1024], BF16)
        wbr = sb("wbr", [128, 4, 256], BF16)
        psum = es.enter_context(nc.psum_tensor("psum", [128, 8 * 512], F32))
        ps = [psum[:, 512 * b:512 * (b + 1)] for b in range(8)]
        bank_free = [None] * 8

        def carve(off, nwords, dt=F32):
            assert off + nwords <= ARENA_W, (off, nwords)
            a = arena[:, off:off + nwords]
            if dt == BF16:
                a = a.bitcast(BF16)
            return a

        class BankSet:
            def __init__(self, ids):
                self.ids = list(ids)
                self.i = 0

            def get(self):
                b = self.ids[self.i % len(self.ids)]
                self.i += 1
                PE.wait(bank_free[b])
                bank_free[b] = None
                return b

        ALLB = BankSet(range(8))

        def barrier(extra=()):
            toks = [q.now() for q in CQ] + list(extra)
            for q in CQ:
                q.wait(*toks)
            return toks

        c_so = SemObj(nc, es, "const")
        dma(SP, ident[:, :], ident_d[:, :], c_so)
        dma(SP, identb[:, :], identb_d[:, :], c_so)
        dma(SP, gains[:, :, :, :].rearrange("p a b c -> p (a b c)"), gains_d[:, :], c_so)
        dma(SP, convw[:, :, :, :].rearrange("p a b c -> p (a b c)"), convw_d[:, :], c_so)
        dma(SP, dmask[:, :, :].rearrange("p a b -> p (a b)"), dmask_d[:, :], c_so)
        dma(SP, tri[:, :], tri_d[:, :], c_so)
        CT = dma(SP, pastc[:, :, :, :].rearrange("p a b c -> p (a b c)"), pastc_d[:, :], c_so)
        DVE.do(nc.vector.memset(ones_bf[:, :], 1.0 / 1024.0))
        DVE.do(nc.vector.memset(epsb[:, :], EPS))
        DVE.do(nc.vector.memset(halo[:, :, :], 0.0))
        barrier([CT])

        class Ring:
            def __init__(self, bufs, name):
                self.bufs = bufs
                self.so = [SemObj(nc, es, f"{name}{i}") for i in range(len(bufs))]
                self.free = [[] for _ in bufs]
                self.i = 0

            def load(self, src, n):
                s = self.i % len(self.bufs)
                self.i += 1
                POOL.wait(self.free[s])
                self.free[s] = []
                tok = dma(POOL, self.bufs[s][:, 0:n], src, self.so[s], max_dma_last_dim=4096)
                return self.bufs[s], tok, s

            def release(self, s, tok):
                self.free[s].append(tok)

        slab_ring = Ring([slabs[:, i, :] for i in range(NSLAB)], "slab")
        wbr_ring = Ring([wbr[:, i, :] for i in range(4)], "wbr")
        A_WDN = 11264
        wdn_ring = Ring([carve(A_WDN + i * 1408, 1408, BF16) for i in range(2)], "wdn")

        evac_i = [0]

        def evac_eng():
            evac_i[0] += 1
            return ACT if evac_i[0] % 2 else DVE

        def copy_on(q, out, in_, scale=None):
            if q is ACT:
                if scale is None:
                    return q.do(nc.scalar.copy(out=out, in_=in_))
                return q.do(nc.scalar.activation(out=out, in_=in_, func=AF.Copy, scale=float(scale)))
            if scale is None:
                return q.do(nc.vector.tensor_copy(out=out, in_=in_))
            return q.do(nc.vector.tensor_scalar(out=out, in0=in_, scalar1=float(scale), scalar2=None, op0=ALU.mult))

        def mm_group(out, pairs, waits=()):
            PE.wait(*waits)
            n = len(pairs)
            inst = None
            for i, (l_, r_) in enumerate(pairs):
                inst = nc.tensor.matmul(out, lhsT=l_, rhs=r_, start=(i == 0), stop=(i == n - 1))
            return PE.do(inst)

        xst = carve(0, 2048).rearrange("p (s f) -> p s f", s=2)
        xst_so = [SemObj(nc, es, f"xst{i}") for i in range(2)]
        yst_so = SemObj(nc, es, "yst")
        st = {}

        def load_x(s):
            barrier([st.get("store_tok")])
            SP.wait([q.now() for q in CQ])
            free = [[], []]
            for tt in range(16):
                sl = tt % 2
                SP.wait(free[sl])
                free[sl] = []
                tl = dma(SP, xst[:, sl, :], x_d[s, tt * 128:(tt + 1) * 128, :], xst_so[sl])
                tp = None
                for half in range(2):
                    b = ALLB.get()
                    PE.wait(tl)
                    inst = None
                    for j in range(4):
                        c = half * 4 + j
                        inst = nc.tensor.transpose(out=ps[b][:, j * 128:(j + 1) * 128],
                                                   in_=xst[:, sl, c * 128:(c + 1) * 128], identity=ident[:, :])
                    tp = PE.do(inst)
                    q = evac_eng()
                    q.wait(tp)
                    tc_ = copy_on(q, xT[:, half * 4:(half + 1) * 4, tt * 128:(tt + 1) * 128],
                                  ps[b].rearrange("p (c t) -> p c t", c=4))
                    bank_free[b] = [tc_]
                free[sl].append(tp)
            barrier()

        def store_x(s):
            barrier()
            free = [[], []]
            last = None
            for tt in range(16):
                sl = tt % 2
                tcs = []
                for half in range(2):
                    b = ALLB.get()
                    inst = None
                    for j in range(4):
                        c = half * 4 + j
                        inst = nc.tensor.transpose(out=ps[b][:, j * 128:(j + 1) * 128],
                                                   in_=xT[:, c, tt * 128:(tt + 1) * 128], identity=ident[:, :])
                    tp = PE.do(inst)
                    q = evac_eng()
                    q.wait(tp, free[sl])
                    tc_ = copy_on(q, xst[:, sl, half * 512:(half + 1) * 512], ps[b])
                    bank_free[b] = [tc_]
                    tcs.append(tc_)
                free[sl] = []
                SP.wait(tcs)
                td = dma(SP, y_d[s, tt * 128:(tt + 1) * 128, :], xst[:, sl, :], yst_so)
                free[sl].append(td)
                last = td
            st["store_tok"] = last
            for q in CQ:
                q.wait(last)

        dbg_so = SemObj(nc, es, "dbg")

        def do_dump():
            barrier()
            SP.wait([q.now() for q in CQ])
            dma(SP, dbg_d[:, 0:ARENA_W], arena[:, :], dbg_so)
            t = dma(SP, dbg_d[:, ARENA_W:ARENA_W + 8192], hTb[:, :].bitcast(F32), dbg_so)
            for q in CQ:
                q.wait(t)

        def rstd_from_sq(sq, waits, sd, rstd):
            b = ALLB.get()
            tp = mm_group(ps[b], [(ones_bf[:, :], sq[:, c, :]) for c in range(8)], waits)
            ACT.wait(tp)
            ta = ACT.do(nc.scalar.activation(out=sd, in_=ps[b], func=AF.Sqrt, bias=epsb[:, 0:1], scale=1.0))
            bank_free[b] = [ta]
            DVE.wait(ta)
            tr = DVE.do(nc.vector.reciprocal(out=rstd, in_=sd))
            return tr, tp

        def pre_norm(l, gi, dst, tg_list, scr_off, nslots=2, extra_waits=()):
            sqb = [carve(scr_off + i * 2560, 2048, BF16).rearrange("p (c t) -> p c t", c=8) for i in range(nslots)]
            sdb = [carve(scr_off + i * 2560 + 2048, 512) for i in range(nslots)]
            free = [list(extra_waits) for _ in range(nslots)]
            t = None
            for j, tg in enumerate(tg_list):
                sl = j % nslots
                ACT.wait(free[sl])
                ts = ACT.do(nc.scalar.activation(out=sqb[sl], in_=xT[:, :, tg * 512:(tg + 1) * 512], func=AF.Square))
                DVE.wait(free[sl])
                free[sl] = []
                tr, tp = rstd_from_sq(sqb[sl], [ts], sdb[sl], sdb[sl])
                DVE.wait(tr, extra_waits)
                for c in range(8):
                    t = DVE.do(nc.vector.scalar_tensor_tensor(
                        out=dst[:, c, j * 512:(j + 1) * 512], in0=xT[:, c, tg * 512:(tg + 1) * 512],
                        scalar=gains[:, l, gi, c:c + 1], in1=sdb[sl], op0=ALU.mult, op1=ALU.mult))
                free[sl] = [t, tp]
            return [t, tp]

        def post_norm_update(l, gi, zparts, tg, waits, sq_off, sd_off):
            sqb = carve(sq_off, 2048, BF16).rearrange("p (c t) -> p c t", c=8)
            sd = carve(sd_off, 512)
            fr = st.get("post_free")
            ACT.wait(waits, fr)
            ts = None
            for (zp, c0) in zparts:
                n = zp.shape[1]
                ts = ACT.do(nc.scalar.activation(out=sqb[:, c0:c0 + n, :], in_=zp, func=AF.Square))
            DVE.wait(fr)
            tr, tp = rstd_from_sq(sqb, [ts], sd, sd)
            DVE.wait(tr, waits, ts)
            t1 = None
            for (zp, c0) in zparts:
                n = zp.shape[1]
                t1 = DVE.do(nc.vector.tensor_tensor(out=zp, in0=zp, in1=sd.unsqueeze(1).to_broadcast([128, n, 512]), op=ALU.mult))
            DVE.wait(t1)
            t = None
            for (zp, c0) in zparts:
                for ci in range(zp.shape[1]):
                    c = c0 + ci
                    t = DVE.do(nc.vector.scalar_tensor_tensor(
                        out=xT[:, c, tg * 512:(tg + 1) * 512], in0=zp[:, ci, :], scalar=gains[:, l, gi, c:c + 1],
                        in1=xT[:, c, tg * 512:(tg + 1) * 512], op0=ALU.mult, op1=ALU.add))
            st["post_free"] = [t, tp]
            return t

        A_V = 0
        A_QK = 9216
        A_P = 11264
        A_U = 12800
        A_RD = 16896
        A_OA = 17408
        A_OB = 3072
        A_MISC = 19456
        A_PN = 9216
        A_MT = 6144
        A_Z = 10240
        A_AB = 14336
        A_SQ = 0
        A_SD = 19456

        def mixer(l):
            Vb = [carve(A_V + i * 3072, 3072, BF16).rearrange("p (t c) -> p t c", t=16) for i in range(3)]
            QKb = [carve(A_QK + i * 1024, 1024, BF16) for i in range(2)]
            Pd = [carve(A_P + i * 128, 128, BF16) for i in range(12)]
            Pm = [carve(A_P + i * 256, 256, BF16) for i in range(6)]
            Ub = [carve(A_U + i * 2048, 2048) for i in range(2)]
            rD = carve(A_RD, 512)
            oaT = carve(A_OA, 2048, BF16).rearrange("p (c t) -> p c t", c=2)
            obT = carve(A_OB, 2048, BF16).rearrange("p (c t) -> p c t", c=2)
            m_ones = carve(A_MISC, 32, BF16)
            gm = carve(A_MISC + 64, 128).rearrange("p (t b) -> p t b", b=8)
            mx = carve(A_MISC + 192, 128).rearrange("p (t b) -> p t b", b=8)
            c1 = carve(A_MISC + 320, 128).rearrange("p (t b) -> p t b", b=8)
            selp = carve(A_MISC + 448, 64, BF16).rearrange("p (t b) -> p t b", b=8)
            km = carve(A_MISC + 512, 8)
            kmb = carve(A_MISC + 520, 4, BF16)

            barrier()
            pre_norm(l, 0, hT, [0, 1, 2, 3], A_PN)
            barrier()
            t_ones = None
            for i in range(3):
                v6 = Vb[i].rearrange("p t (b c) -> p t b c", c=64)
                t_ones = DVE.do(nc.vector.memset(v6[:, :, 1:5:3, :], 1.0))
            PE.wait(t_ones)

            def load_chunk(ci):
                return slab_ring.load(win_d[l, ci, :, :], 1024)

            def proj_fm(ci, consume):
                buf, tokw, s = load_chunk(ci)
                tp = None
                for tg in range(4):
                    b = ALLB.get()
                    tp = mm_group(ps[b], [(buf[:, kc * 128:(kc + 1) * 128], hT[:, kc, tg * 512:(tg + 1) * 512]) for kc in range(8)], [tokw])
                    tcs = consume(tg, b, tp)
                    bank_free[b] = list(tcs)
                slab_ring.release(s, tp)

            def v_proj(G, V, free_toks):
                c0 = 12 + 2 * G if G < 3 else 22
                b0, t0, s0 = load_chunk(c0)
                b1, t1, s1 = load_chunk(c0 + 1)
                d = DILS[G] if G < 3 else 1
                L = SEQ // d
                toks = []
                tp = None
                for tau in range(16):
                    r = (128 * tau) // L
                    i0 = (128 * tau) % L
                    start = i0 * d + r
                    b = ALLB.get()
                    PE.wait(t0, t1)
                    inst = None
                    assert s1 == s0 + 1
                    for kc in range(8):
                        w0 = b0[:, kc * 128:(kc + 1) * 128]
                        rhs = bass.AP(tensor=w0.tensor, offset=w0.offset, ap=[list(w0.ap[0]), [1024, 2], [1, 128]])
                        inst = nc.tensor.matmul(ps[b][:, 0:256], lhsT=hT[:, kc, start:start + 127 * d + 1:d],
                                                rhs=rhs, start=(kc == 0), stop=(kc == 7))
                    tp = PE.do(inst)
                    q = evac_eng()
                    q.wait(tp, free_toks)
                    v6 = V[:, tau, :].rearrange("p (b c) -> p b c", c=64)
                    p4 = ps[b][:, 0:256].rearrange("p (b c) -> p b c", c=64)
                    q.wait(t_ones)
                    copy_on(q, v6[:, 0:3:2, :], p4[:, 0:2, :])
                    tc_ = copy_on(q, v6[:, 3:6:2, :], p4[:, 2:4, :])
                    bank_free[b] = [tc_]
                    toks.append(tc_)
                slab_ring.release(s0, tp)
                slab_ring.release(s1, tp)
                return toks

            def V_aug(V, tile, hd):
                v = V[:, tile, hd * 64:(hd + 1) * 64]
                return v, m_ones

            SB_ = BankSet([0, 1, 2, 3])
            UBA = BankSet([4, 6])
            UBB = BankSet([5, 7])

            VOFF = (0, 64, 192, 256)

            def pv_mm(out, V, tile, hd, rhs, start, stop):
                return nc.tensor.matmul(out, lhsT=V[:, tile, VOFF[hd]:VOFF[hd] + 128], rhs=rhs, start=start, stop=stop)

            def dil_attention(g, sp, Qs, Ks, qk_toks, V, v_toks, first, acc_free):
                d = DILS[g]
                L = SEQ // d
                nblk = L // 128
                SKEW = 2
                ptoks, pslot = {}, {}
                pfree = st.setdefault("pd_free", [[] for _ in range(12)])
                ucur = {}
                out_toks = []
                lastS = None
                for step in range(16 + SKEW):
                    if step < 16:
                        kt = step
                        nq = 256 if (kt % nblk) < nblk - 1 else 128
                        for h in range(2):
                            hp = 64 * h
                            bS = SB_.get()
                            PE.wait(qk_toks)
                            tS = PE.do(nc.tensor.matmul(ps[bS][:, 0:nq], lhsT=Ks[hp:hp + 64, kt * 128:(kt + 1) * 128],
                                                        rhs=Qs[hp:hp + 64, kt * 128:kt * 128 + nq], start=True, stop=True))
                            lastS = tS
                            sl = st.get("pd_i", 0) % 12
                            st["pd_i"] = st.get("pd_i", 0) + 1
                            ACT.wait(tS, pfree[sl])
                            pfree[sl] = []
                            tE = ACT.do(nc.scalar.activation(out=Pd[sl][:, 0:nq], in_=ps[bS][:, 0:nq], func=AF.Exp))
                            bank_free[bS] = [tE]
                            DVE.wait(tE)
                            head = g * 4 + 2 * sp + h
                            tM = DVE.do(nc.vector.tensor_tensor(out=Pd[sl][:, 0:nq], in0=Pd[sl][:, 0:nq],
                                                                in1=dmask[:, head, 0:nq], op=ALU.mult))
                            ptoks[(h, kt)] = tM
                            pslot[(h, kt)] = sl
                    if step >= SKEW:
                        qb = step - SKEW
                        n = qb % nblk
                        for h in range(2):
                            if qb % 4 == 0:
                                ucur[h] = (UBA if h == 0 else UBB).get()
                            ub = ucur[h]
                            col = (qb % 4) * 128
                            hd = 2 * sp + h
                            lst = []
                            if n > 0:
                                lst.append((qb - 1, pslot[(h, qb - 1)], 128))
                            lst.append((qb, pslot[(h, qb)], 0))
                            PE.wait(v_toks, ptoks[(h, qb)], ptoks.get((h, qb - 1)))
                            inst = None
                            for i, (ktile, sl, pc) in enumerate(lst):
                                inst = pv_mm(ps[ub][:, col:col + 128], V, ktile, hd, Pd[sl][:, pc:pc + 128],
                                             start=(i == 0), stop=(i == len(lst) - 1))
                            tU = PE.do(inst)
                            if n > 0:
                                pfree[pslot[(h, qb - 1)]].append(tU)
                            if n == nblk - 1:
                                pfree[pslot[(h, qb)]].append(tU)
                            if qb % 4 == 3:
                                m = qb // 4
                                U = Ub[h]
                                if d == 1:
                                    dst = U[:, m * 512:(m + 1) * 512]
                                    src = ps[ub]
                                elif d == 4:
                                    dst = U[:, m:SEQ:4]
                                    src = ps[ub]
                                else:
                                    dst = U.rearrange("p (i r) -> p r i", r=16)[:, 4 * m:4 * m + 4, :]
                                    src = ps[ub].rearrange("p (r i) -> p r i", r=4)
                                if first:
                                    ACT.wait(tU, acc_free)
                                    te = copy_on(ACT, dst, src)
                                else:
                                    DVE.wait(tU, st.get("u_last"))
                                    te = DVE.do(nc.vector.tensor_tensor(out=dst, in0=dst, in1=src, op=ALU.add))
                                bank_free[ub] = [te]
                                out_toks.append(te)
                st["u_last"] = out_toks
                return out_toks, lastS

            def normalize_sb(U, u_toks, slot, oT):
                toks = []
                ur = (slot % 2) * 64
                dr = 64 - ur
                for tg in range(4):
                    DVE.wait(u_toks, st.get("rd_free"))
                    t1 = DVE.do(nc.vector.reciprocal(out=rD[ur:ur + 64, :], in_=U[dr:dr + 64, tg * 512:(tg + 1) * 512]))
                    DVE.wait(t1)
                    t2 = DVE.do(nc.vector.tensor_tensor(out=oT[ur:ur + 64, slot // 2, tg * 512:(tg + 1) * 512],
                                                        in0=U[ur:ur + 64, tg * 512:(tg + 1) * 512], in1=rD[ur:ur + 64, :], op=ALU.mult))
                    st["rd_free"] = [t2]
                    toks.append(t2)
                return toks

            vtoks = [v_proj(g, Vb[g], None) for g in range(3)]
            qk_free = None
            acc_free = None
            last_pv = None
            for sp in range(2):
                u_toks = None
                for g in range(3):
                    d = DILS[g]
                    qk_toks = []
                    for which in range(2):
                        ci = which * 6 + g * 2 + sp
                        dstb = QKb[which]

                        def consume(tg, b, tp, dstb=dstb, which=which, d=d):
                            q = evac_eng()
                            q.wait(tp, qk_free)
                            n_i = 512 // d
                            if d == 1:
                                o_ap = dstb[:, tg * 512:(tg + 1) * 512]
                                i_ap = ps[b]
                            else:
                                o_ap = dstb.rearrange("p (r i) -> p r i", r=d)[:, :, tg * n_i:(tg + 1) * n_i]
                                i_ap = ps[b].rearrange("p (i r) -> p r i", r=d)
                            t = copy_on(q, o_ap, i_ap, scale=(SCALE if which == 0 else None))
                            qk_toks.append(t)
                            return [t]
                        proj_fm(ci, consume)
                    u_toks, lastS = dil_attention(g, sp, QKb[0], QKb[1], qk_toks, Vb[g], vtoks[g], g == 0, acc_free)
                    qk_free = [lastS]
                    last_pv = PE.now()
                nt = []
                for h in range(2):
                    nt += normalize_sb(Ub[h], u_toks, 2 * sp + h, oaT)
                acc_free = nt
            if mixer_stage < 2:
                return

            AUG = [QKb[0], QKb[1], carve(A_V + 6144, 1024, BF16), carve(A_V + 7168, 1024, BF16)]
            barrier()
            vm_toks = v_proj(3, Vb[0], None)
            if "aug_so" not in st:
                st["aug_so"] = [SemObj(nc, es, f"aug{i}") for i in range(4)]
            aug_so = st["aug_so"]
            for hpair in range(2):
                QA, KA, QB, KB = AUG
                heads = (2 * hpair, 2 * hpair + 1)
                SP.wait([q.now() for q in CQ])
                tt_ = [dma(SP, QA[64:128, :], qaug_d[heads[0], :, :], aug_so[0]),
                       dma(SP, KA[64:128, :], kaug_d[heads[0], :, :], aug_so[1]),
                       dma(SP, QB[0:64, :], qaug_d[heads[1], :, :], aug_so[2]),
                       dma(SP, KB[0:64, :], kaug_d[heads[1], :, :], aug_so[3])]
                qk_toks = []
                for which in range(2):
                    ci = 18 + 2 * which + hpair
                    dA, dB = (QA, QB) if which == 0 else (KA, KB)

                    def consume(tg, b, tp, dA=dA, dB=dB, which=which):
                        sc = SCALE if which == 0 else None
                        ACT.wait(tp)
                        ta = copy_on(ACT, dA[0:64, tg * 512:(tg + 1) * 512], ps[b][0:64, :], scale=sc)
                        DVE.wait(tp)
                        tb = copy_on(DVE, dB[64:128, tg * 512:(tg + 1) * 512], ps[b][64:128, :], scale=sc)
                        qk_toks.extend([ta, tb])
                        return [ta, tb]
                    proj_fm(ci, consume)
                for h in range(2):
                    Qx, Kx = (QA, KA) if h == 0 else (QB, KB)
                    r0 = 0 if h == 0 else 64
                    e0 = 64 if h == 0 else 0
                    DVE.wait(qk_toks, st.get("km_free"))
                    tk1 = DVE.do(nc.vector.tensor_reduce(out=km[r0:r0 + 64, :], in_=Kx[r0:r0 + 64, :].rearrange("p (b k) -> p b k", b=8),
                                                         axis=AX.X, op=ALU.add))
                    DVE.wait(tk1)
                    tk2 = DVE.do(nc.vector.tensor_scalar(out=kmb[r0:r0 + 64, :], in0=km[r0:r0 + 64, :], scalar1=1.0 / 256.0, scalar2=None, op0=ALU.mult))
                    bg = SB_.get()
                    PE.wait(tk2, qk_toks)
                    inst = None
                    for tau in range(16):
                        inst = nc.tensor.matmul(ps[bg][:, tau * 8:(tau + 1) * 8], lhsT=Qx[r0:r0 + 64, tau * 128:(tau + 1) * 128],
                                                rhs=kmb[r0:r0 + 64, :], start=True, stop=True)
                    tg_ = PE.do(inst)
                    st["km_free"] = [tg_]
                    DVE.wait(tg_, st.get("gm_free"))
                    t1 = DVE.do(nc.vector.tensor_tensor(out=gm, in0=ps[bg][:, 0:128].rearrange("p (t b) -> p t b", b=8),
                                                        in1=pastc[:, 0, :, :], op=ALU.add))
                    bank_free[bg] = [t1]
                    DVE.wait(t1)
                    t2 = None
                    for tau in range(16):
                        t2 = DVE.do(nc.vector.max(out=mx[:, tau, :], in_=gm[:, tau, :]))
                    DVE.wait(t2)
                    t3 = DVE.do(nc.vector.tensor_tensor(out=c1, in0=gm, in1=mx[:, :, 2:3].to_broadcast([128, 16, 8]), op=ALU.is_ge))
                    DVE.wait(t3)
                    t4 = DVE.do(nc.vector.tensor_tensor(out=c1, in0=c1, in1=pastc[:, 1, :, :], op=ALU.mult))
                    DVE.wait(t4)
                    t5 = DVE.do(nc.vector.tensor_tensor(out=c1, in0=c1, in1=pastc[:, 2, :, :], op=ALU.add))
                    DVE.wait(t5)
                    t6 = DVE.do(nc.vector.tensor_scalar(out=selp, in0=c1, scalar1=-1.0, scalar2=-NEG, op0=ALU.add, op1=ALU.mult))
                    b1 = SB_.get()
                    b2 = SB_.get()
                    PE.wait(t6)
                    inst = None
                    for tau in range(16):
                        bb = b1 if tau < 8 else b2
                        pso = ps[bb].bitcast(BF16)
                        inst = nc.tensor.transpose(out=pso[0:8, (tau % 8) * 128:(tau % 8 + 1) * 128], in_=selp[:, tau, :], identity=identb[:, :])
                    tT = PE.do(inst)
                    st["gm_free"] = [tT]
                    ACT.wait(tT, tt_)
                    ta = ACT.do(nc.scalar.copy(out=Qx[e0:e0 + 8, 0:1024], in_=ps[b1].bitcast(BF16)[0:8, :]))
                    DVE.wait(tT, tt_)
                    tb = DVE.do(nc.vector.tensor_copy(out=Qx[e0:e0 + 8, 1024:2048], in_=ps[b2].bitcast(BF16)[0:8, :]))
                    bank_free[b1] = [ta]
                    bank_free[b2] = [tb]
                    qk_toks.extend([ta, tb])
                units = [(qg, kt, h) for qg in range(4) for kt in range(4 * qg + 4) for h in range(2)]
                SKEW = 4
                info = {}
                pfree = st.setdefault("pm_free", [[] for _ in range(6)])
                ucur = {}
                for i in range(len(units) + SKEW):
                    if i < len(units):
                        qg, kt, h = units[i]
                        Qx, Kx = (QA, KA) if h == 0 else (QB, KB)
                        c0 = max(0, kt - 4 * qg) * 128
                        nq = 512 - c0
                        bS = SB_.get()
                        PE.wait(qk_toks, tt_)
                        tS = PE.do(nc.tensor.matmul(ps[bS][:, 0:nq], lhsT=Kx[:, kt * 128:(kt + 1) * 128],
                                                    rhs=Qx[:, qg * 512 + c0:(qg + 1) * 512], start=True, stop=True))
                        sl = st.get("pm_i", 0) % 6
                        st["pm_i"] = st.get("pm_i", 0) + 1
                        ACT.wait(tS, pfree[sl])
                        pfree[sl] = []
                        tE = ACT.do(nc.scalar.activation(out=Pm[sl][:, 0:nq], in_=ps[bS][:, 0:nq], func=AF.Exp))
                        bank_free[bS] = [tE]
                        if kt >= 4 * qg:
                            DVE.wait(tE)
                            tE = DVE.do(nc.vector.tensor_tensor(out=Pm[sl][:, 0:128], in0=Pm[sl][:, 0:128], in1=tri[:, :], op=ALU.mult))
                        info[i] = (tE, sl, c0, nq)
                    if i >= SKEW:
                        j = i - SKEW
                        qg, kt, h = units[j]
                        tE, sl, c0, nq = info.pop(j)
                        if kt == 0:
                            ucur[h] = (UBA if h == 0 else UBB).get()
                        ub = ucur[h]
                        hd = 2 * hpair + h
                        PE.wait(tE, vm_toks)
                        last = (kt == 4 * qg + 3)
                        inst = pv_mm(ps[ub][:, c0:512], Vb[0], kt, hd, Pm[sl][:, 0:nq], start=(kt == 0), stop=last)
                        tU = PE.do(inst)
                        pfree[sl].append(tU)
                        if last:
                            DVE.wait(tU, st.get("rd_free"))
                            ur = (hd % 2) * 64
                            dr = 64 - ur
                            t1 = DVE.do(nc.vector.reciprocal(out=rD[ur:ur + 64, :], in_=ps[ub][dr:dr + 64, :]))
                            DVE.wait(t1)
                            t2 = DVE.do(nc.vector.tensor_tensor(out=obT[ur:ur + 64, hd // 2, qg * 512:(qg + 1) * 512],
                                                                in0=ps[ub][ur:ur + 64, :], in1=rD[ur:ur + 64, :], op=ALU.mult))
                            st["rd_free"] = [t2]
                            bank_free[ub] = [t2]
                barrier()
            if dump == "attn":
                do_dump()
                return
            if mixer_stage < 3:
                return

            mT = carve(A_MT, 4096, BF16).rearrange("p (c t) -> p c t", c=8)
            z = carve(A_Z, 4096).rearrange("p (c t) -> p c t", c=8)
            AB = [carve(A_AB + i * 512, 512) for i in range(4)]
            barrier()
            ab_free = [[], [], [], []]
            for hf in range(2):
                mt_toks = []
                for oc in range(8):
                    bga, tga, sga = load_chunk(24 + oc)
                    bgb, tgb, sgb = load_chunk(32 + oc)
                    bwa, twa, swa = wbr_ring.load(wbrd_d[l, oc, :, :], 256)
                    bwm, twm, swm = wbr_ring.load(wbrm_d[l, oc, :, :], 256)
                    tp = None
                    for tgi in range(2):
                        tg = 2 * hf + tgi
                        tsl = slice(tg * 512, (tg + 1) * 512)
                        b_ga = ALLB.get()
                        t_ga = mm_group(ps[b_ga], [(bga[:, kc * 128:(kc + 1) * 128], hT[:, kc, tsl]) for kc in range(8)], [tga])
                        b_gb = ALLB.get()
                        t_gb = mm_group(ps[b_gb], [(bgb[:, kc * 128:(kc + 1) * 128], hT[:, kc, tsl]) for kc in range(8)], [tgb])
                        b_ya = ALLB.get()
                        t_ya = mm_group(ps[b_ya], [(bwa[:, kc * 128:(kc + 1) * 128], oaT[:, kc, tsl]) for kc in range(2)], [twa])
                        b_yb = ALLB.get()
                        t_yb = mm_group(ps[b_yb], [(bwm[:, kc * 128:(kc + 1) * 128], obT[:, kc, tsl]) for kc in range(2)], [twm])
                        tp = t_yb
                        ia = (tgi % 2) * 2
                        A_, B_ = AB[ia], AB[ia + 1]
                        ACT.wait(t_ga, ab_free[ia])
                        ab_free[ia] = []
                        s1 = ACT.do(nc.scalar.activation(out=A_, in_=ps[b_ga], func=AF.Sigmoid))
                        bank_free[b_ga] = [s1]
                        ACT.wait(t_gb, ab_free[ia + 1])
                        ab_free[ia + 1] = []
                        s2 = ACT.do(nc.scalar.activation(out=B_, in_=ps[b_gb], func=AF.Sigmoid))
                        bank_free[b_gb] = [s2]
                        DVE.wait(s1, t_ya)
                        m1 = DVE.do(nc.vector.tensor_tensor(out=A_, in0=A_, in1=ps[b_ya], op=ALU.mult))
                        bank_free[b_ya] = [m1]
                        DVE.wait(s2, t_yb)
                        m2 = DVE.do(nc.vector.tensor_tensor(out=B_, in0=B_, in1=ps[b_yb], op=ALU.mult))
                        bank_free[b_yb] = [m2]
                        DVE.wait(m1, m2, st.get("mt_free"))
                        m3 = DVE.do(nc.vector.tensor_tensor(out=mT[:, oc, tgi * 512:(tgi + 1) * 512], in0=A_, in1=B_, op=ALU.add))
                        ab_free[ia] = [m3]
                        ab_free[ia + 1] = [m3]
                        mt_toks.append(m3)
                    slab_ring.release(sga, tp)
                    slab_ring.release(sgb, tp)
                    wbr_ring.release(swa, tp)
                    wbr_ring.release(swm, tp)
                for tgi in range(2):
                    tg = 2 * hf + tgi
                    zt = []
                    for oc in range(8):
                        bw, tw, sw = slab_ring.load(wout_d[l, oc, :, :], 1024)
                        b = ALLB.get()
                        tp = mm_group(ps[b], [(bw[:, kc * 128:(kc + 1) * 128], mT[:, kc, tgi * 512:(tgi + 1) * 512]) for kc in range(8)], [tw, mt_toks])
                        slab_ring.release(sw, tp)
                        q = evac_eng()
                        q.wait(tp, st.get("post_free"))
                        tc_ = copy_on(q, z[:, oc, :], ps[b])
                        bank_free[b] = [tc_]
                        zt.append(tc_)
                    post_norm_update(l, 1, [(z, 0)], tg, zt, A_SQ, A_SD)
                st["mt_free"] = [PE.now()]
            barrier()

        F_UT = 0
        F_WDN = 11264
        F_A = 14328
        F_T1 = 16384
        F_ZB = 14336

        def ffn(l):
            h2h = [hTb[:, i * 8192:(i + 1) * 8192].rearrange("p (c t) -> p c t", c=8) for i in range(2)]
            zA = hTb[:, 0:8192].bitcast(F32).rearrange("p (c t) -> p c t", c=4)
            zB = carve(F_ZB, 4096).rearrange("p (c t) -> p c t", c=4)
            uT = carve(F_UT, 11264, BF16).rearrange("p (c t) -> p c t", c=NFC)
            a_full = [carve(F_A + i * 1028, 1028) for i in range(2)]
            t1b = [carve(F_T1 + i * 1024, 1024) for i in range(2)]
            barrier()
            st["post_free"] = None
            tn = [pre_norm(l, 2, h2h[0], [0, 1], 0, nslots=2), None]
            afree = [[], []]
            t1free = [[[], []], [[], []]]
            ut_free = None
            zb_free = None
            for hf in range(2):
                h2 = h2h[hf]
                cur = {}
                wl = {}

                def stage1(fc, tgi):
                    slot = fc % 2
                    a_ = a_full[slot]
                    t1_ = t1b[slot][:, tgi * 512:(tgi + 1) * 512]
                    if tgi == 0:
                        cur["g"] = slab_ring.load(wgate_d[l, fc, :, :], 1024)
                        cur["u"] = slab_ring.load(wup_d[l, fc, :, :], 1024)
                    bg, tg_w, sg = cur["g"]
                    bu, tu_w, su = cur["u"]
                    tsl = slice(tgi * 512, (tgi + 1) * 512)
                    b_a = ALLB.get()
                    t_a = mm_group(ps[b_a], [(bg[:, kc * 128:(kc + 1) * 128], h2[:, kc, tsl]) for kc in range(8)], [tg_w, tn[hf]])
                    b_u = ALLB.get()
                    t_u = mm_group(ps[b_u], [(bu[:, kc * 128:(kc + 1) * 128], h2[:, kc, tsl]) for kc in range(8)], [tu_w])
                    if tgi == 1:
                        slab_ring.release(sg, t_u)
                        slab_ring.release(su, t_u)
                    ACT.wait(t_a, t1free[slot][tgi], zb_free)
                    th = None
                    if tgi == 0:
                        ACT.wait(afree[slot], st.get("halo_tok"))
                        afree[slot] = []
                        th = ACT.do(nc.scalar.copy(out=a_[:, 0:2], in_=halo[:, fc, :]))
                    t1free[slot][tgi] = []
                    tc_ = ACT.do(nc.scalar.copy(out=a_[:, 2 + 512 * tgi:514 + 512 * tgi], in_=ps[b_a]))
                    tt1 = ACT.do(nc.scalar.activation(out=t1_, in_=ps[b_a], func=AF.Identity,
                                                      bias=convw[:, l, fc, 3:4], scale=convw[:, l, fc, 2:3]))
                    bank_free[b_a] = [tt1]
                    if tgi == 1 and hf == 0:
                        ACT.wait(tc_)
                        th2 = ACT.do(nc.scalar.copy(out=halo[:, fc, :], in_=a_[:, 1024:1026]))
                        st["halo_tok"] = [th2]
                    DVE.wait(tt1, tc_, th, zb_free)
                    d1 = DVE.do(nc.vector.scalar_tensor_tensor(out=t1_, in0=a_[:, 1 + 512 * tgi:513 + 512 * tgi], scalar=convw[:, l, fc, 1:2],
                                                               in1=t1_, op0=ALU.mult, op1=ALU.add))
                    DVE.wait(d1)
                    d2 = DVE.do(nc.vector.scalar_tensor_tensor(out=t1_, in0=a_[:, 512 * tgi:512 + 512 * tgi], scalar=convw[:, l, fc, 0:1],
                                                               in1=t1_, op0=ALU.mult, op1=ALU.add))
                    return dict(fc=fc, tgi=tgi, slot=slot, t1=t1_, b_u=b_u, t_u=t_u, d2=d2, tsl=tsl)

                def stage2(u):
                    ACT.wait(u["d2"])
                    g1 = ACT.do(nc.scalar.activation(out=u["t1"], in_=u["t1"], func=AF.Gelu_apprx_tanh))
                    DVE.wait(g1, u["t_u"], ut_free)
                    u1 = DVE.do(nc.vector.tensor_tensor(out=uT[:, u["fc"], u["tsl"]], in0=u["t1"], in1=ps[u["b_u"]], op=ALU.mult))
                    bank_free[u["b_u"]] = [u1]
                    t1free[u["slot"]][u["tgi"]] = [u1]
                    if u["tgi"] == 1:
                        afree[u["slot"]] = [u1]
                    return u1

                units = [(fc, tgi) for fc in range(NFC) for tgi in range(2)]
                LAG = 1
                pend = {}
                last_u1 = None
                for i in range(len(units) + LAG):
                    if i < len(units):
                        fc, tgi = units[i]
                        pend[i] = stage1(fc, tgi)
                        if hf == 0 and fc == 6 and tgi == 0:
                            tn[1] = pre_norm(l, 2, h2h[1], [2, 3], F_WDN, nslots=1)
                        if fc == NFC - 3 and tgi == 0:
                            POOL.wait(tn[1] if hf == 0 else st.get("post_free"))
                            for oc in range(2):
                                wl[oc] = wdn_ring.load(wdown_d[l, oc, :, :], DFF)
                    if i >= LAG:
                        last_u1 = stage2(pend.pop(i - LAG))
                zb_free = None
                if hf == 1:
                    DVE.wait(st.get("halo_tok"))
                    tz = DVE.do(nc.vector.memset(halo[:, :, :], 0.0))
                    st["halo_tok"] = [tz]
                zt = []
                tp = None
                for oc in range(8):
                    bw, tw, sw = wl.pop(oc)
                    for tgi in range(2):
                        b = ALLB.get()
                        tp = mm_group(ps[b], [(bw[:, kc * 128:(kc + 1) * 128], uT[:, kc, tgi * 512:(tgi + 1) * 512]) for kc in range(NFC)],
                                      [tw, last_u1])
                        q = evac_eng()
                        q.wait(tp, st.get("post_free"))
                        dst = zA[:, oc, tgi * 512:(tgi + 1) * 512] if oc < 4 else zB[:, oc - 4, tgi * 512:(tgi + 1) * 512]
                        tc_ = copy_on(q, dst, ps[b])
                        bank_free[b] = [tc_]
                        zt.append(tc_)
                    wdn_ring.release(sw, tp)
                    if oc + 2 < 8:
                        wl[oc + 2] = wdn_ring.load(wdown_d[l, oc + 2, :, :], DFF)
                ut_free = [tp]
                for tgi in range(2):
                    tsl = slice(tgi * 512, (tgi + 1) * 512)
                    post_norm_update(l, 3, [(zA[:, :, tsl], 0), (zB[:, :, tsl], 4)], 2 * hf + tgi, zt + [tp], F_WDN, F_WDN + 2048)
                zb_free = st["post_free"]

        for s in range(n_seq):
            load_x(s)
            for l in range(n_layers):
                if do_mixer:
                    mixer(l)
                if do_ffn:
                    ffn(l)
            store_x(s)
        for q in CQ + (SP,):
            q.wait(st["store_tok"])
            q.flush()
    return nc


def _slopes():
    i = np.arange(1, 17, dtype=np.float32)
    return np.exp2(-8.0 * i / 16).astype(np.float32)


def _chunk_layout(w, kdim):
    nk = kdim // 128
    noc = w.shape[1] // 128
    return np.ascontiguousarray(w.reshape(nk, 128, noc, 128).transpose(2, 1, 0, 3).reshape(noc, 128, nk * 128))


def _bf16_hi_lo(a):
    hi = a.astype(ml_dtypes.bfloat16)
    lo = (a - hi.astype(np.float32)).astype(ml_dtypes.bfloat16)
    return hi, lo


def _constants():
    sl = _slopes()
    bf = ml_dtypes.bfloat16
    k = np.arange(128)[:, None].astype(np.float32)
    q = np.arange(256)[None, :].astype(np.float32)
    delta = q - k
    valid = (delta >= 0) & (delta <= 128)
    dmask = np.zeros((128, 12, 256), np.float32)
    for g in range(3):
        for j in range(4):
            h = g * 4 + j
            dmask[:, h, :] = np.where(valid, np.exp(-sl[h] * DILS[g] * np.where(valid, delta, 0.0)), 0.0)
    tri = (np.arange(128)[None, :] >= np.arange(128)[:, None]).astype(np.float32)
    t = np.arange(SEQ)
    qaug = np.zeros((4, 64, SEQ), np.float32)
    kaug = np.zeros((4, 64, SEQ), np.float32)
    qaug_b = np.zeros((4, 64, SEQ), bf)
    kaug_b = np.zeros((4, 64, SEQ), bf)
    for hm in range(4):
        m = np.float32(sl[12 + hm])
        for b in range(8):
            ind = (t // 256 == b).astype(np.float32)
            A = (-m * (t - 256 * b)).astype(np.float32)
            hi, lo = _bf16_hi_lo(A)
            for base in (0, 8, 16):
                kaug_b[hm, base + b] = ind.astype(bf)
            qaug_b[hm, 8 + b] = hi
            qaug_b[hm, 16 + b] = lo
        kp = (m * (t % 256)).astype(np.float32)
        hi, lo = _bf16_hi_lo(kp)
        kaug_b[hm, 24] = hi
        kaug_b[hm, 25] = lo
        qaug_b[hm, 24] = np.ones(SEQ, bf)
        qaug_b[hm, 25] = np.ones(SEQ, bf)
    pastc = np.zeros((128, 3, 16, 8), np.float32)
    for tau in range(16):
        for b in range(8):
            pastc[:, 0, tau, b] = 0.0 if b < tau // 2 else -1e30
            pastc[:, 1, tau, b] = 1.0 if b < tau // 2 else 0.0
            pastc[:, 2, tau, b] = 1.0 if b == tau // 2 else 0.0
    return {
        "dmask": np.ascontiguousarray(dmask.reshape(128, 12 * 256)).astype(bf),
        "tri": tri.astype(bf),
        "qaug_t": qaug_b, "kaug_t": kaug_b,
        "pastc": np.ascontiguousarray(pastc.reshape(128, 384)),
        "ident": np.eye(128, dtype=np.float32),
        "identb": np.eye(128, dtype=np.float32).astype(bf),
    }


def prep_shared(inp, n_layers=2):
    f = lambda a: np.asarray(a, dtype=np.float32)
    sh = {}
    sh["w_in_r"] = np.stack([_chunk_layout(f(inp["w_in"][l]), 1024) for l in range(2)])
    sh["w_brd_r"] = np.stack([_chunk_layout(f(inp["w_branch_dil"][l]), 256) for l in range(2)])
    sh["w_brm_r"] = np.stack([_chunk_layout(f(inp["w_branch_moba"][l]), 256) for l in range(2)])
    sh["w_out_r"] = np.stack([_chunk_layout(f(inp["w_out"][l]), 1024) for l in range(2)])
    sh["w_gate_r"] = np.stack([_chunk_layout(f(inp["w_ffn_gate"][l]), 1024) for l in range(2)])
    sh["w_up_r"] = np.stack([_chunk_layout(f(inp["w_ffn_up"][l]), 1024) for l in range(2)])
    sh["w_down_r"] = np.stack([_chunk_layout(f(inp["w_ffn_down"][l]), DFF) for l in range(2)])
    g = np.stack([f(inp["mix_norm_pre"]), f(inp["mix_norm_post"]), f(inp["ffn_norm_pre"]), f(inp["ffn_norm_post"])], axis=1)
    sh["gains"] = np.ascontiguousarray(g.reshape(2, 4, 8, 128).transpose(3, 0, 1, 2).reshape(128, 64))
    cw = np.concatenate([f(inp["ffn_conv_w"]), f(inp["ffn_conv_b"])[:, None, :]], axis=1)
    sh["convw"] = np.ascontiguousarray(cw.reshape(2, 4, NFC, 128).transpose(3, 0, 2, 1).reshape(128, 2 * NFC * 4))
    sh.update(_constants())
    return sh


_NC_CACHE = {}


def kernel(**inputs):
    x = np.asarray(inputs["x"], dtype=np.float32)
    sh = prep_shared(inputs)
    if "nc" not in _NC_CACHE:
        _NC_CACHE["nc"] = build()
    nc = _NC_CACHE["nc"]
    in_maps = []
    for c in range(NCORES):
        m = dict(sh)
        m["x"] = np.ascontiguousarray(x[2 * c:2 * c + 2])
        in_maps.append(m)
    res = run_bass_kernel_spmd(nc, in_maps, core_ids=list(range(NCORES)))
    out = np.concatenate([np.asarray(r["y"], dtype=np.float32) for r in res.results], axis=0)
    return out
```

```python
import contextlib
import numpy as np
import ml_dtypes
import concourse.bass as bass
import concourse.mybir as mybir
from concourse.bass_utils import run_bass_kernel_spmd

F32 = mybir.dt.float32
BF16 = mybir.dt.bfloat16
AF = mybir.ActivationFunctionType
ALU = mybir.AluOpType
AX = mybir.AxisListType

SEQ = 2048
D = 1024
DFF = 2816
NFC = 22
NCORES = 8
SCALE = 0.125
EPS = 1e-6
DILS = (1, 4, 16)
NEG = -30000.0


class SemObj:
    def __init__(self, nc, es, name):
        self.sem = es.enter_context(nc.semaphore(name))
        self.n = 0


class Q:
    def __init__(self, nc, es, eng, name, attach=False):
        self.eng = eng
        self.so = SemObj(nc, es, "q_" + name)
        self.seen = {}
        self.name = name
        self.attach = attach
        self.pending = None

    def _collect(self, toks, acc):
        for t in toks:
            if t is None:
                continue
            if isinstance(t, (list, tuple)) and not (len(t) == 2 and isinstance(t[0], SemObj)):
                self._collect(t, acc)
                continue
            so, v = t
            if acc.get(so, 0) < v:
                acc[so] = v

    def wait(self, *toks):
        acc = {}
        self._collect(toks, acc)
        for so, v in acc.items():
            if self.seen.get(so, 0) >= v:
                continue
            self.seen[so] = v
            if self.pending is not None:
                self.eng.wait_ge(self.pending[0].sem, self.pending[1])
                self.pending = None
            if self.attach:
                self.pending = (so, v)
            else:
                self.eng.wait_ge(so.sem, v)

    def flush(self):
        if self.pending is not None:
            self.eng.wait_ge(self.pending[0].sem, self.pending[1])
            self.pending = None

    def do(self, inst):
        if self.pending is not None:
            inst._wait_ge(self.pending[0].sem, self.pending[1])
            self.pending = None
        inst.then_inc(self.so.sem, 1)
        self.so.n += 1
        return (self.so, self.so.n)

    def now(self):
        return (self.so, self.so.n) if self.so.n > 0 else None


def build(n_seq=2, n_layers=2, do_mixer=True, do_ffn=True, mixer_stage=99, dump=False):
    nc = bass.Bass("TRN2", target_bir_lowering=False)

    def dt_in(name, shape, dt=F32):
        return nc.dram_tensor(name, list(shape), dt, kind="ExternalInput").ap()

    x_d = dt_in("x", [2, SEQ, D])
    win_d = dt_in("w_in_r", [2, 40, 128, 1024])
    wbrd_d = dt_in("w_brd_r", [2, 8, 128, 256])
    wbrm_d = dt_in("w_brm_r", [2, 8, 128, 256])
    wout_d = dt_in("w_out_r", [2, 8, 128, 1024])
    wgate_d = dt_in("w_gate_r", [2, NFC, 128, 1024])
    wup_d = dt_in("w_up_r", [2, NFC, 128, 1024])
    wdown_d = dt_in("w_down_r", [2, 8, 128, DFF])
    gains_d = dt_in("gains", [128, 2 * 4 * 8])
    convw_d = dt_in("convw", [128, 2 * NFC * 4])
    dmask_d = dt_in("dmask", [128, 12 * 256], BF16)
    tri_d = dt_in("tri", [128, 128], BF16)
    qaug_d = dt_in("qaug_t", [4, 64, SEQ], BF16)
    kaug_d = dt_in("kaug_t", [4, 64, SEQ], BF16)
    pastc_d = dt_in("pastc", [128, 3 * 128])
    ident_d = dt_in("ident", [128, 128])
    identb_d = dt_in("identb", [128, 128], BF16)
    y_d = nc.dram_tensor("y", [2, SEQ, D], F32, kind="ExternalOutput").ap()
    dbg_d = nc.dram_tensor("dbg", [128, 21504 + 8192], F32, kind="ExternalOutput").ap() if dump else None

    es = contextlib.ExitStack()
    with es:
        def sb(name, shape, dt):
            return es.enter_context(nc.sbuf_tensor("s_" + name, list(shape), dt))

        PE = Q(nc, es, nc.tensor, "pe")
        ACT = Q(nc, es, nc.scalar, "act", attach=True)
        DVE = Q(nc, es, nc.vector, "dve", attach=True)
        POOL = Q(nc, es, nc.gpsimd, "pool")
        SP = Q(nc, es, nc.sync, "sp")
        CQ = (PE, ACT, DVE)

        def dma(q, out, in_, so, **kw):
            q.eng.dma_start(out=out, in_=in_, **kw).then_inc(so.sem, 16)
            so.n += 16
            return (so, so.n)

        xT = sb("xT", [128, 8, SEQ], F32)
        hTb = sb("hTb", [128, 8 * SEQ], BF16)
        hT = hTb[:, :].rearrange("p (c t) -> p c t", c=8)
        ident = sb("ident", [128, 128], F32)
        identb = sb("identb", [128, 128], BF16)
        ones_bf = sb("ones_bf", [128, 128], BF16)
        gains = sb("gains", [128, 2, 4, 8], F32)
        convw = sb("convw", [128, 2, NFC, 4], F32)
        dmask = sb("dmask", [128, 12, 256], BF16)
        tri = sb("tri", [128, 128], BF16)
        pastc = sb("pastc", [128, 3, 16, 8], F32)
        epsb = sb("epsb", [128, 1], F32)
        halo = sb("halo", [128, NFC, 2], F32)
        ARENA_W = 21504
        arena = sb("arena", [128, ARENA_W], F32)
        NSLAB = 6
        slabs = sb("slabs", [128, NSLAB, 1024], BF16)
        wbr = sb("wbr", [128, 4, 256], BF16)
        psum = es.enter_context(nc.psum_tensor("psum", [128, 8 * 512], F32))
        ps = [psum[:, 512 * b:512 * (b + 1)] for b in range(8)]
        bank_free = [None] * 8

        def carve(off, nwords, dt=F32):
            assert off + nwords <= ARENA_W, (off, nwords)
            a = arena[:, off:off + nwords]
            if dt == BF16:
                a = a.bitcast(BF16)
            return a

        class BankSet:
            def __init__(self, ids):
                self.ids = list(ids)
                self.i = 0

            def get(self):
                b = self.ids[self.i % len(self.ids)]
                self.i += 1
                PE.wait(bank_free[b])
                bank_free[b] = None
                return b

        ALLB = BankSet(range(8))

        def barrier(extra=()):
            toks = [q.now() for q in CQ] + list(extra)
            for q in CQ:
                q.wait(*toks)
            return toks

        c_so = SemObj(nc, es, "const")
        dma(SP, ident[:, :], ident_d[:, :], c_so)
        dma(SP, identb[:, :], identb_d[:, :], c_so)
        dma(SP, gains[:, :, :, :].rearrange("p a b c -> p (a b c)"), gains_d[:, :], c_so)
        dma(SP, convw[:, :, :, :].rearrange("p a b c -> p (a b c)"), convw_d[:, :], c_so)
        dma(SP, dmask[:, :, :].rearrange("p a b -> p (a b)"), dmask_d[:, :], c_so)
        dma(SP, tri[:, :], tri_d[:, :], c_so)
        CT = dma(SP, pastc[:, :, :, :].rearrange("p a b c -> p (a b c)"), pastc_d[:, :], c_so)
        DVE.do(nc.vector.memset(ones_bf[:, :], 1.0 / 1024.0))
        DVE.do(nc.vector.memset(epsb[:, :], EPS))
        DVE.do(nc.vector.memset(halo[:, :, :], 0.0))
        barrier([CT])

        class Ring:
            def __init__(self, bufs, name):
                self.bufs = bufs
                self.so = [SemObj(nc, es, f"{name}{i}") for i in range(len(bufs))]
                self.free = [[] for _ in bufs]
                self.i = 0

            def load(self, src, n):
                s = self.i % len(self.bufs)
                self.i += 1
                POOL.wait(self.free[s])
                self.free[s] = []
                tok = dma(POOL, self.bufs[s][:, 0:n], src, self.so[s], max_dma_last_dim=4096)
                return self.bufs[s], tok, s

            def release(self, s, tok):
                self.free[s].append(tok)

        slab_ring = Ring([slabs[:, i, :] for i in range(NSLAB)], "slab")
        wbr_ring = Ring([wbr[:, i, :] for i in range(4)], "wbr")
        A_WDN = 11264
        wdn_ring = Ring([carve(A_WDN + i * 1408, 1408, BF16) for i in range(2)], "wdn")

        evac_i = [0]

        def evac_eng():
            evac_i[0] += 1
            return ACT if evac_i[0] % 2 else DVE

        def copy_on(q, out, in_, scale=None):
            if q is ACT:
                if scale is None:
                    return q.do(nc.scalar.copy(out=out, in_=in_))
                return q.do(nc.scalar.activation(out=out, in_=in_, func=AF.Copy, scale=float(scale)))
            if scale is None:
                return q.do(nc.vector.tensor_copy(out=out, in_=in_))
            return q.do(nc.vector.tensor_scalar(out=out, in0=in_, scalar1=float(scale), scalar2=None, op0=ALU.mult))

        def mm_group(out, pairs, waits=()):
            PE.wait(*waits)
            n = len(pairs)
            inst = None
            for i, (l_, r_) in enumerate(pairs):
                inst = nc.tensor.matmul(out, lhsT=l_, rhs=r_, start=(i == 0), stop=(i == n - 1))
            return PE.do(inst)

        NXS = 6
        xst = carve(0, NXS * 1024).rearrange("p (s f) -> p s f", s=NXS)
        xst_so = [SemObj(nc, es, f"xst{i}") for i in range(NXS)]
        yst_so = SemObj(nc, es, "yst")
        st = {}

        def load_x(s):
            barrier([st.get("store_tok")])
            SP.wait([q.now() for q in CQ])
            free = [[] for _ in range(NXS)]
            for tt in range(16):
                sl = tt % NXS
                SP.wait(free[sl])
                free[sl] = []
                tl = dma(SP, xst[:, sl, :], x_d[s, tt * 128:(tt + 1) * 128, :], xst_so[sl])
                tp = None
                for half in range(2):
                    b = ALLB.get()
                    PE.wait(tl)
                    inst = None
                    for j in range(4):
                        c = half * 4 + j
                        inst = nc.tensor.transpose(out=ps[b][:, j * 128:(j + 1) * 128],
                                                   in_=xst[:, sl, c * 128:(c + 1) * 128], identity=ident[:, :])
                    tp = PE.do(inst)
                    q = evac_eng()
                    q.wait(tp)
                    tc_ = copy_on(q, xT[:, half * 4:(half + 1) * 4, tt * 128:(tt + 1) * 128],
                                  ps[b].rearrange("p (c t) -> p c t", c=4))
                    bank_free[b] = [tc_]
                free[sl].append(tp)
            barrier()

        def store_x(s):
            barrier()
            free = [[] for _ in range(NXS)]
            last = None
            for tt in range(16):
                sl = tt % NXS
                tcs = []
                for half in range(2):
                    b = ALLB.get()
                    inst = None
                    for j in range(4):
                        c = half * 4 + j
                        inst = nc.tensor.transpose(out=ps[b][:, j * 128:(j + 1) * 128],
                                                   in_=xT[:, c, tt * 128:(tt + 1) * 128], identity=ident[:, :])
                    tp = PE.do(inst)
                    q = evac_eng()
                    q.wait(tp, free[sl])
                    tc_ = copy_on(q, xst[:, sl, half * 512:(half + 1) * 512], ps[b])
                    bank_free[b] = [tc_]
                    tcs.append(tc_)
                free[sl] = []
                SP.wait(tcs)
                td = dma(SP, y_d[s, tt * 128:(tt + 1) * 128, :], xst[:, sl, :], yst_so)
                free[sl].append(td)
                last = td
            st["store_tok"] = last
            for q in CQ:
                q.wait(last)

        dbg_so = SemObj(nc, es, "dbg")

        def do_dump():
            barrier()
            SP.wait([q.now() for q in CQ])
            dma(SP, dbg_d[:, 0:ARENA_W], arena[:, :], dbg_so)
            t = dma(SP, dbg_d[:, ARENA_W:ARENA_W + 8192], hTb[:, :].bitcast(F32), dbg_so)
            for q in CQ:
                q.wait(t)

        def rstd_from_sq(sq, waits, sd, rstd):
            b = ALLB.get()
            tp = mm_group(ps[b], [(ones_bf[:, :], sq[:, c, :]) for c in range(8)], waits)
            ACT.wait(tp)
            ta = ACT.do(nc.scalar.activation(out=sd, in_=ps[b], func=AF.Sqrt, bias=epsb[:, 0:1], scale=1.0))
            bank_free[b] = [ta]
            DVE.wait(ta)
            tr = DVE.do(nc.vector.reciprocal(out=rstd, in_=sd))
            return tr, tp

        def pre_norm(l, gi, dst, tg_list, scr_off, nslots=2, extra_waits=()):
            sqb = [carve(scr_off + i * 2560, 2048, BF16).rearrange("p (c t) -> p c t", c=8) for i in range(nslots)]
            sdb = [carve(scr_off + i * 2560 + 2048, 512) for i in range(nslots)]
            free = [list(extra_waits) for _ in range(nslots)]
            t = None
            for j, tg in enumerate(tg_list):
                sl = j % nslots
                ACT.wait(free[sl])
                ts = ACT.do(nc.scalar.activation(out=sqb[sl], in_=xT[:, :, tg * 512:(tg + 1) * 512], func=AF.Square))
                DVE.wait(free[sl])
                free[sl] = []
                tr, tp = rstd_from_sq(sqb[sl], [ts], sdb[sl], sdb[sl])
                DVE.wait(tr, extra_waits)
                for c in range(8):
                    t = DVE.do(nc.vector.scalar_tensor_tensor(
                        out=dst[:, c, j * 512:(j + 1) * 512], in0=xT[:, c, tg * 512:(tg + 1) * 512],
                        scalar=gains[:, l, gi, c:c + 1], in1=sdb[sl], op0=ALU.mult, op1=ALU.mult))
                free[sl] = [t, tp]
            return [t, tp]

        def post_norm_update(l, gi, zparts, tg, waits, sq_off, sd_off):
            sqb = carve(sq_off, 2048, BF16).rearrange("p (c t) -> p c t", c=8)
            sd = carve(sd_off, 512)
            fr = st.get("post_free")
            ACT.wait(waits, fr)
            ts = None
            for (zp, c0) in zparts:
                n = zp.shape[1]
                ts = ACT.do(nc.scalar.activation(out=sqb[:, c0:c0 + n, :], in_=zp, func=AF.Square))
            DVE.wait(fr)
            tr, tp = rstd_from_sq(sqb, [ts], sd, sd)
            DVE.wait(tr, waits, ts)
            t1 = None
            for (zp, c0) in zparts:
                n = zp.shape[1]
                t1 = DVE.do(nc.vector.tensor_tensor(out=zp, in0=zp, in1=sd.unsqueeze(1).to_broadcast([128, n, 512]), op=ALU.mult))
            DVE.wait(t1)
            t = None
            for (zp, c0) in zparts:
                for ci in range(zp.shape[1]):
                    c = c0 + ci
                    t = DVE.do(nc.vector.scalar_tensor_tensor(
                        out=xT[:, c, tg * 512:(tg + 1) * 512], in0=zp[:, ci, :], scalar=gains[:, l, gi, c:c + 1],
                        in1=xT[:, c, tg * 512:(tg + 1) * 512], op0=ALU.mult, op1=ALU.add))
            st["post_free"] = [t, tp]
            return t

        A_V = 0
        A_QK = 9216
        A_P = 11264
        A_U = 12800
        A_RD = 16896
        A_OA = 17408
        A_OB = 3072
        A_MISC = 19456
        A_PN = 9216
        A_MT = 6144
        A_Z = 10240
        A_AB = 14336
        A_SQ = 0
        A_SD = 19456

        def mixer(l):
            Vb = [carve(A_V + i * 3072, 3072, BF16).rearrange("p (t c) -> p t c", t=16) for i in range(3)]
            QKb = [carve(A_QK + i * 1024, 1024, BF16) for i in range(2)]
            Pd = [carve(A_P + i * 128, 128, BF16) for i in range(12)]
            Pm = [carve(A_P + i * 256, 256, BF16) for i in range(6)]
            Ub = [carve(A_U + i * 2048, 2048) for i in range(2)]
            rD = carve(A_RD, 512)
            oaT = carve(A_OA, 2048, BF16).rearrange("p (c t) -> p c t", c=2)
            obT = carve(A_OB, 2048, BF16).rearrange("p (c t) -> p c t", c=2)
            m_ones = carve(A_MISC, 32, BF16)
            gm = carve(A_MISC + 64, 128).rearrange("p (t b) -> p t b", b=8)
            mx = carve(A_MISC + 192, 128).rearrange("p (t b) -> p t b", b=8)
            c1 = carve(A_MISC + 320, 128).rearrange("p (t b) -> p t b", b=8)
            selp = carve(A_MISC + 448, 64, BF16).rearrange("p (t b) -> p t b", b=8)
            km = carve(A_MISC + 512, 8)
            kmb = carve(A_MISC + 520, 4, BF16)

            barrier()
            hT_toks = pre_norm(l, 0, hT, [0, 1, 2, 3], A_PN)
            PE.wait(hT_toks)
            t_ones = None
            for i in range(3):
                v6 = Vb[i].rearrange("p t (b c) -> p t b c", c=64)
                t_ones = DVE.do(nc.vector.memset(v6[:, :, 1:5:3, :], 1.0))
            PE.wait(t_ones)

            def load_chunk(ci):
                return slab_ring.load(win_d[l, ci, :, :], 1024)

            def proj_fm(ci, consume):
                buf, tokw, s = load_chunk(ci)
                tp = None
                for tg in range(4):
                    b = ALLB.get()
                    tp = mm_group(ps[b], [(buf[:, kc * 128:(kc + 1) * 128], hT[:, kc, tg * 512:(tg + 1) * 512]) for kc in range(8)], [tokw])
                    tcs = consume(tg, b, tp)
                    bank_free[b] = list(tcs)
                slab_ring.release(s, tp)

            def v_proj(G, V, free_toks):
                c0 = 12 + 2 * G if G < 3 else 22
                b0, t0, s0 = load_chunk(c0)
                b1, t1, s1 = load_chunk(c0 + 1)
                d = DILS[G] if G < 3 else 1
                L = SEQ // d
                toks = []
                tp = None
                for tau in range(16):
                    r = (128 * tau) // L
                    i0 = (128 * tau) % L
                    start = i0 * d + r
                    b = ALLB.get()
                    PE.wait(t0, t1)
                    inst = None
                    assert s1 == s0 + 1
                    for kc in range(8):
                        w0 = b0[:, kc * 128:(kc + 1) * 128]
                        rhs = bass.AP(tensor=w0.tensor, offset=w0.offset, ap=[list(w0.ap[0]), [1024, 2], [1, 128]])
                        inst = nc.tensor.matmul(ps[b][:, 0:256], lhsT=hT[:, kc, start:start + 127 * d + 1:d],
                                                rhs=rhs, start=(kc == 0), stop=(kc == 7))
                    tp = PE.do(inst)
                    q = evac_eng()
                    q.wait(tp, free_toks)
                    v6 = V[:, tau, :].rearrange("p (b c) -> p b c", c=64)
                    p4 = ps[b][:, 0:256].rearrange("p (b c) -> p b c", c=64)
                    q.wait(t_ones)
                    copy_on(q, v6[:, 0:3:2, :], p4[:, 0:2, :])
                    tc_ = copy_on(q, v6[:, 3:6:2, :], p4[:, 2:4, :])
                    bank_free[b] = [tc_]
                    toks.append(tc_)
                slab_ring.release(s0, tp)
                slab_ring.release(s1, tp)
                return toks

            def V_aug(V, tile, hd):
                v = V[:, tile, hd * 64:(hd + 1) * 64]
                return v, m_ones

            SB_ = BankSet([0, 1, 2, 3])
            UBA = BankSet([4, 6])
            UBB = BankSet([5, 7])

            VOFF = (0, 64, 192, 256)

            def pv_mm(out, V, tile, hd, rhs, start, stop):
                return nc.tensor.matmul(out, lhsT=V[:, tile, VOFF[hd]:VOFF[hd] + 128], rhs=rhs, start=start, stop=stop)

            def dil_attention(g, sp, Qs, Ks, qk_toks, V, v_toks, first, acc_free):
                d = DILS[g]
                L = SEQ // d
                nblk = L // 128
                SKEW = 2
                ptoks, pslot = {}, {}
                pfree = st.setdefault("pd_free", [[] for _ in range(12)])
                ucur = {}
                out_toks = []
                lastS = None
                for step in range(16 + SKEW):
                    if step < 16:
                        kt = step
                        nq = 256 if (kt % nblk) < nblk - 1 else 128
                        for h in range(2):
                            hp = 64 * h
                            bS = SB_.get()
                            PE.wait(qk_toks)
                            tS = PE.do(nc.tensor.matmul(ps[bS][:, 0:nq], lhsT=Ks[hp:hp + 64, kt * 128:(kt + 1) * 128],
                                                        rhs=Qs[hp:hp + 64, kt * 128:kt * 128 + nq], start=True, stop=True))
                            lastS = tS
                            sl = st.get("pd_i", 0) % 12
                            st["pd_i"] = st.get("pd_i", 0) + 1
                            ACT.wait(tS, pfree[sl])
                            pfree[sl] = []
                            tE = ACT.do(nc.scalar.activation(out=Pd[sl][:, 0:nq], in_=ps[bS][:, 0:nq], func=AF.Exp))
                            bank_free[bS] = [tE]
                            DVE.wait(tE)
                            head = g * 4 + 2 * sp + h
                            tM = DVE.do(nc.vector.tensor_tensor(out=Pd[sl][:, 0:nq], in0=Pd[sl][:, 0:nq],
                                                                in1=dmask[:, head, 0:nq], op=ALU.mult))
                            ptoks[(h, kt)] = tM
                            pslot[(h, kt)] = sl
                    if step >= SKEW:
                        qb = step - SKEW
                        n = qb % nblk
                        for h in range(2):
                            if qb % 4 == 0:
                                ucur[h] = (UBA if h == 0 else UBB).get()
                            ub = ucur[h]
                            col = (qb % 4) * 128
                            hd = 2 * sp + h
                            lst = []
                            if n > 0:
                                lst.append((qb - 1, pslot[(h, qb - 1)], 128))
                            lst.append((qb, pslot[(h, qb)], 0))
                            PE.wait(v_toks, ptoks[(h, qb)], ptoks.get((h, qb - 1)))
                            inst = None
                            for i, (ktile, sl, pc) in enumerate(lst):
                                inst = pv_mm(ps[ub][:, col:col + 128], V, ktile, hd, Pd[sl][:, pc:pc + 128],
                                             start=(i == 0), stop=(i == len(lst) - 1))
                            tU = PE.do(inst)
                            if n > 0:
                                pfree[pslot[(h, qb - 1)]].append(tU)
                            if n == nblk - 1:
                                pfree[pslot[(h, qb)]].append(tU)
                            if qb % 4 == 3:
                                m = qb // 4
                                U = Ub[h]
                                if d == 1:
                                    dst = U[:, m * 512:(m + 1) * 512]
                                    src = ps[ub]
                                elif d == 4:
                                    dst = U[:, m:SEQ:4]
                                    src = ps[ub]
                                else:
                                    dst = U.rearrange("p (i r) -> p r i", r=16)[:, 4 * m:4 * m + 4, :]
                                    src = ps[ub].rearrange("p (r i) -> p r i", r=4)
                                if first:
                                    ACT.wait(tU, acc_free)
                                    te = copy_on(ACT, dst, src)
                                else:
                                    DVE.wait(tU, st.get("u_last"))
                                    te = DVE.do(nc.vector.tensor_tensor(out=dst, in0=dst, in1=src, op=ALU.add))
                                bank_free[ub] = [te]
                                out_toks.append(te)
                st["u_last"] = out_toks
                return out_toks, lastS

            def normalize_sb(U, u_toks, slot, oT):
                toks = []
                ur = (slot % 2) * 64
                dr = 64 - ur
                for tg in range(4):
                    DVE.wait(u_toks, st.get("rd_free"))
                    t1 = DVE.do(nc.vector.reciprocal(out=rD[ur:ur + 64, :], in_=U[dr:dr + 64, tg * 512:(tg + 1) * 512]))
                    DVE.wait(t1)
                    t2 = DVE.do(nc.vector.tensor_tensor(out=oT[ur:ur + 64, slot // 2, tg * 512:(tg + 1) * 512],
                                                        in0=U[ur:ur + 64, tg * 512:(tg + 1) * 512], in1=rD[ur:ur + 64, :], op=ALU.mult))
                    st["rd_free"] = [t2]
                    toks.append(t2)
                return toks

            vtoks = [v_proj(g, Vb[g], None) for g in range(3)]
            qk_free = None
            acc_free = None
            last_pv = None
            for sp in range(2):
                u_toks = None
                for g in range(3):
                    d = DILS[g]
                    qk_toks = []
                    for which in range(2):
                        ci = which * 6 + g * 2 + sp
                        dstb = QKb[which]

                        def consume(tg, b, tp, dstb=dstb, which=which, d=d):
                            q = evac_eng()
                            q.wait(tp, qk_free)
                            n_i = 512 // d
                            if d == 1:
                                o_ap = dstb[:, tg * 512:(tg + 1) * 512]
                                i_ap = ps[b]
                            else:
                                o_ap = dstb.rearrange("p (r i) -> p r i", r=d)[:, :, tg * n_i:(tg + 1) * n_i]
                                i_ap = ps[b].rearrange("p (i r) -> p r i", r=d)
                            t = copy_on(q, o_ap, i_ap, scale=(SCALE if which == 0 else None))
                            qk_toks.append(t)
                            return [t]
                        proj_fm(ci, consume)
                    u_toks, lastS = dil_attention(g, sp, QKb[0], QKb[1], qk_toks, Vb[g], vtoks[g], g == 0, acc_free)
                    qk_free = [lastS]
                    last_pv = PE.now()
                if sp == 0:
                    nt = []
                    for h in range(2):
                        nt += normalize_sb(Ub[h], u_toks, 2 * sp + h, oaT)
                    acc_free = nt

            def deferred_norm():
                for h in range(2):
                    normalize_sb(Ub[h], u_toks, 2 + h, oaT)

            AUG = [QKb[0], QKb[1], carve(A_V + 6144, 1024, BF16), carve(A_V + 7168, 1024, BF16)]
            vm_toks = v_proj(3, Vb[0], None)
            if "aug_so" not in st:
                st["aug_so"] = [SemObj(nc, es, f"aug{i}") for i in range(4)]
            aug_so = st["aug_so"]
            for hpair in range(2):
                QA, KA, QB, KB = AUG
                heads = (2 * hpair, 2 * hpair + 1)
                SP.wait([q.now() for q in CQ])
                tt_ = [dma(SP, QA[64:128, :], qaug_d[heads[0], :, :], aug_so[0]),
                       dma(SP, KA[64:128, :], kaug_d[heads[0], :, :], aug_so[1]),
                       dma(SP, QB[0:64, :], qaug_d[heads[1], :, :], aug_so[2]),
                       dma(SP, KB[0:64, :], kaug_d[heads[1], :, :], aug_so[3])]
                qk_toks = []
                for which in range(2):
                    ci = 18 + 2 * which + hpair
                    dA, dB = (QA, QB) if which == 0 else (KA, KB)

                    def consume(tg, b, tp, dA=dA, dB=dB, which=which):
                        sc = SCALE if which == 0 else None
                        ACT.wait(tp)
                        ta = copy_on(ACT, dA[0:64, tg * 512:(tg + 1) * 512], ps[b][0:64, :], scale=sc)
                        DVE.wait(tp)
                        tb = copy_on(DVE, dB[64:128, tg * 512:(tg + 1) * 512], ps[b][64:128, :], scale=sc)
                        qk_toks.extend([ta, tb])
                        return [ta, tb]
                    proj_fm(ci, consume)
                if hpair == 0:
                    deferred_norm()
                HX = [(QA, KA, 0, 64), (QB, KB, 64, 0)]
                gmh = [carve(A_MISC + 64 + h * 448, 128).rearrange("p (t b) -> p t b", b=8) for h in range(2)]
                mxh = [carve(A_MISC + 192 + h * 448, 128).rearrange("p (t b) -> p t b", b=8) for h in range(2)]
                c1h = [carve(A_MISC + 320 + h * 448, 128).rearrange("p (t b) -> p t b", b=8) for h in range(2)]
                sph = [carve(A_MISC + 448 + h * 448, 64, BF16).rearrange("p (t b) -> p t b", b=8) for h in range(2)]
                km = carve(A_MISC + 1024, 8)
                kmb = carve(A_MISC + 1032, 4, BF16)
                tk = [None, None]
                for h, (Qx, Kx, r0, e0) in enumerate(HX):
                    DVE.wait(qk_toks, st.get("km_free"))
                    tk[h] = DVE.do(nc.vector.tensor_reduce(out=km[r0:r0 + 64, :], in_=Kx[r0:r0 + 64, :].rearrange("p (b k) -> p b k", b=8),
                                                           axis=AX.X, op=ALU.add))
                for h, (Qx, Kx, r0, e0) in enumerate(HX):
                    DVE.wait(tk[h])
                    tk[h] = DVE.do(nc.vector.tensor_scalar(out=kmb[r0:r0 + 64, :], in0=km[r0:r0 + 64, :], scalar1=1.0 / 256.0, scalar2=None, op0=ALU.mult))
                tgm = [None, None]
                bgs = [None, None]
                for h, (Qx, Kx, r0, e0) in enumerate(HX):
                    bgs[h] = ALLB.get()
                    PE.wait(tk[h], qk_toks)
                    inst = None
                    for tau in range(16):
                        inst = nc.tensor.matmul(ps[bgs[h]][:, tau * 8:(tau + 1) * 8], lhsT=Qx[r0:r0 + 64, tau * 128:(tau + 1) * 128],
                                                rhs=kmb[r0:r0 + 64, :], start=True, stop=True)
                    tgm[h] = PE.do(inst)
                st["km_free"] = list(tgm)
                tc = [None, None]
                for h in range(2):
                    DVE.wait(tgm[h], st.get("gm_free"))
                    tc[h] = DVE.do(nc.vector.tensor_tensor(out=gmh[h], in0=ps[bgs[h]][:, 0:128].rearrange("p (t b) -> p t b", b=8),
                                                           in1=pastc[:, 0, :, :], op=ALU.add))
                    bank_free[bgs[h]] = [tc[h]]
                for h in range(2):
                    DVE.wait(tc[h])
                    for tau in range(16):
                        tc[h] = DVE.do(nc.vector.max(out=mxh[h][:, tau, :], in_=gmh[h][:, tau, :]))
                for h in range(2):
                    DVE.wait(tc[h])
                    tc[h] = DVE.do(nc.vector.tensor_tensor(out=c1h[h], in0=gmh[h], in1=mxh[h][:, :, 2:3].to_broadcast([128, 16, 8]), op=ALU.is_ge))
                for h in range(2):
                    DVE.wait(tc[h])
                    tc[h] = DVE.do(nc.vector.tensor_tensor(out=c1h[h], in0=c1h[h], in1=pastc[:, 1, :, :], op=ALU.mult))
                for h in range(2):
                    DVE.wait(tc[h])
                    tc[h] = DVE.do(nc.vector.tensor_tensor(out=c1h[h], in0=c1h[h], in1=pastc[:, 2, :, :], op=ALU.add))
                for h in range(2):
                    DVE.wait(tc[h])
                    tc[h] = DVE.do(nc.vector.tensor_scalar(out=sph[h], in0=c1h[h], scalar1=-1.0, scalar2=-NEG, op0=ALU.add, op1=ALU.mult))
                tT = [None, None]
                bb12 = [None, None]
                for h in range(2):
                    b1 = ALLB.get()
                    b2 = ALLB.get()
                    bb12[h] = (b1, b2)
                    PE.wait(tc[h])
                    inst = None
                    for tau in range(16):
                        bb = b1 if tau < 8 else b2
                        pso = ps[bb].bitcast(BF16)
                        inst = nc.tensor.transpose(out=pso[0:8, (tau % 8) * 128:(tau % 8 + 1) * 128], in_=sph[h][:, tau, :], identity=identb[:, :])
                    tT[h] = PE.do(inst)
                st["gm_free"] = list(tT)
                for h, (Qx, Kx, r0, e0) in enumerate(HX):
                    b1, b2 = bb12[h]
                    ACT.wait(tT[h], tt_)
                    ta = ACT.do(nc.scalar.copy(out=Qx[e0:e0 + 8, 0:1024], in_=ps[b1].bitcast(BF16)[0:8, :]))
                    DVE.wait(tT[h], tt_)
                    tb = DVE.do(nc.vector.tensor_copy(out=Qx[e0:e0 + 8, 1024:2048], in_=ps[b2].bitcast(BF16)[0:8, :]))
                    bank_free[b1] = [ta]
                    bank_free[b2] = [tb]
                    qk_toks.extend([ta, tb])
                units = [(qg, kt, h) for qg in range(4) for kt in range(4 * qg + 4) for h in range(2)]
                SKEW = 4
                info = {}
                pfree = st.setdefault("pm_free", [[] for _ in range(6)])
                ucur = {}
                for i in range(len(units) + SKEW):
                    if i < len(units):
                        qg, kt, h = units[i]
                        Qx, Kx = (QA, KA) if h == 0 else (QB, KB)
                        c0 = max(0, kt - 4 * qg) * 128
                        nq = 512 - c0
                        bS = SB_.get()
                        PE.wait(qk_toks, tt_)
                        tS = PE.do(nc.tensor.matmul(ps[bS][:, 0:nq], lhsT=Kx[:, kt * 128:(kt + 1) * 128],
                                                    rhs=Qx[:, qg * 512 + c0:(qg + 1) * 512], start=True, stop=True))
                        sl = st.get("pm_i", 0) % 6
                        st["pm_i"] = st.get("pm_i", 0) + 1
                        ACT.wait(tS, pfree[sl])
                        pfree[sl] = []
                        tE = ACT.do(nc.scalar.activation(out=Pm[sl][:, 0:nq], in_=ps[bS][:, 0:nq], func=AF.Exp))
                        bank_free[bS] = [tE]
                        if kt >= 4 * qg:
                            DVE.wait(tE)
                            tE = DVE.do(nc.vector.tensor_tensor(out=Pm[sl][:, 0:128], in0=Pm[sl][:, 0:128], in1=tri[:, :], op=ALU.mult))
                        info[i] = (tE, sl, c0, nq)
                    if i >= SKEW:
                        j = i - SKEW
                        qg, kt, h = units[j]
                        tE, sl, c0, nq = info.pop(j)
                        if kt == 0:
                            ucur[h] = (UBA if h == 0 else UBB).get()
                        ub = ucur[h]
                        hd = 2 * hpair + h
                        PE.wait(tE, vm_toks)
                        last = (kt == 4 * qg + 3)
                        inst = pv_mm(ps[ub][:, c0:512], Vb[0], kt, hd, Pm[sl][:, 0:nq], start=(kt == 0), stop=last)
                        tU = PE.do(inst)
                        pfree[sl].append(tU)
                        if last:
                            DVE.wait(tU, st.get("rd_free"))
                            ur = (hd % 2) * 64
                            dr = 64 - ur
                            t1 = DVE.do(nc.vector.reciprocal(out=rD[ur:ur + 64, :], in_=ps[ub][dr:dr + 64, :]))
                            DVE.wait(t1)
                            t2 = DVE.do(nc.vector.tensor_tensor(out=obT[ur:ur + 64, hd // 2, qg * 512:(qg + 1) * 512],
                                                                in0=ps[ub][ur:ur + 64, :], in1=rD[ur:ur + 64, :], op=ALU.mult))
                            st["rd_free"] = [t2]
                            bank_free[ub] = [t2]
            if dump == "attn":
                do_dump()
                return
            if mixer_stage < 3:
                return

            mT = carve(A_MT, 4096, BF16).rearrange("p (c t) -> p c t", c=8)
            z = carve(A_Z, 4096).rearrange("p (c t) -> p c t", c=8)
            AB = [carve(A_AB + i * 512, 512) for i in range(4)]
            barrier()
            ab_free = [[], [], [], []]
            for hf in range(2):
                mt_toks = []
                for oc in range(8):
                    bga, tga, sga = load_chunk(24 + oc)
                    bgb, tgb, sgb = load_chunk(32 + oc)
                    bwa, twa, swa = wbr_ring.load(wbrd_d[l, oc, :, :], 256)
                    bwm, twm, swm = wbr_ring.load(wbrm_d[l, oc, :, :], 256)
                    tp = None
                    for tgi in range(2):
                        tg = 2 * hf + tgi
                        tsl = slice(tg * 512, (tg + 1) * 512)
                        b_ga = ALLB.get()
                        t_ga = mm_group(ps[b_ga], [(bga[:, kc * 128:(kc + 1) * 128], hT[:, kc, tsl]) for kc in range(8)], [tga])
                        b_gb = ALLB.get()
                        t_gb = mm_group(ps[b_gb], [(bgb[:, kc * 128:(kc + 1) * 128], hT[:, kc, tsl]) for kc in range(8)], [tgb])
                        b_ya = ALLB.get()
                        t_ya = mm_group(ps[b_ya], [(bwa[:, kc * 128:(kc + 1) * 128], oaT[:, kc, tsl]) for kc in range(2)], [twa])
                        b_yb = ALLB.get()
                        t_yb = mm_group(ps[b_yb], [(bwm[:, kc * 128:(kc + 1) * 128], obT[:, kc, tsl]) for kc in range(2)], [twm])
                        tp = t_yb
                        ia = (tgi % 2) * 2
                        A_, B_ = AB[ia], AB[ia + 1]
                        ACT.wait(t_ga, ab_free[ia])
                        ab_free[ia] = []
                        s1 = ACT.do(nc.scalar.activation(out=A_, in_=ps[b_ga], func=AF.Sigmoid))
                        bank_free[b_ga] = [s1]
                        ACT.wait(t_gb, ab_free[ia + 1])
                        ab_free[ia + 1] = []
                        s2 = ACT.do(nc.scalar.activation(out=B_, in_=ps[b_gb], func=AF.Sigmoid))
                        bank_free[b_gb] = [s2]
                        DVE.wait(s1, t_ya)
                        m1 = DVE.do(nc.vector.tensor_tensor(out=A_, in0=A_, in1=ps[b_ya], op=ALU.mult))
                        bank_free[b_ya] = [m1]
                        DVE.wait(s2, t_yb)
                        m2 = DVE.do(nc.vector.tensor_tensor(out=B_, in0=B_, in1=ps[b_yb], op=ALU.mult))
                        bank_free[b_yb] = [m2]
                        DVE.wait(m1, m2, st.get("mt_free"))
                        m3 = DVE.do(nc.vector.tensor_tensor(out=mT[:, oc, tgi * 512:(tgi + 1) * 512], in0=A_, in1=B_, op=ALU.add))
                        ab_free[ia] = [m3]
                        ab_free[ia + 1] = [m3]
                        mt_toks.append(m3)
                    slab_ring.release(sga, tp)
                    slab_ring.release(sgb, tp)
                    wbr_ring.release(swa, tp)
                    wbr_ring.release(swm, tp)
                for tgi in range(2):
                    tg = 2 * hf + tgi
                    zt = []
                    for oc in range(8):
                        bw, tw, sw = slab_ring.load(wout_d[l, oc, :, :], 1024)
                        b = ALLB.get()
                        tp = mm_group(ps[b], [(bw[:, kc * 128:(kc + 1) * 128], mT[:, kc, tgi * 512:(tgi + 1) * 512]) for kc in range(8)], [tw, mt_toks])
                        slab_ring.release(sw, tp)
                        q = evac_eng()
                        q.wait(tp, st.get("post_free"))
                        tc_ = copy_on(q, z[:, oc, :], ps[b])
                        bank_free[b] = [tc_]
                        zt.append(tc_)
                    post_norm_update(l, 1, [(z, 0)], tg, zt, A_SQ, A_SD)
                st["mt_free"] = [PE.now()]
            barrier()

        F_UT = 0
        F_WDN = 11264
        F_A = 14328
        F_T1 = 16384
        F_ZB = 14336

        def ffn(l):
            h2h = [hTb[:, i * 8192:(i + 1) * 8192].rearrange("p (c t) -> p c t", c=8) for i in range(2)]
            zA = hTb[:, 0:8192].bitcast(F32).rearrange("p (c t) -> p c t", c=4)
            zB = carve(F_ZB, 4096).rearrange("p (c t) -> p c t", c=4)
            uT = carve(F_UT, 11264, BF16).rearrange("p (c t) -> p c t", c=NFC)
            a_full = [carve(F_A + i * 1028, 1028) for i in range(2)]
            t1b = [carve(F_T1 + i * 1024, 1024) for i in range(2)]
            barrier()
            st["post_free"] = None
            tn = [pre_norm(l, 2, h2h[0], [0, 1], 0, nslots=2), None]
            afree = [[], []]
            t1free = [[[], []], [[], []]]
            ut_free = None
            zb_free = None
            for hf in range(2):
                h2 = h2h[hf]
                cur = {}
                wl = {}

                def stage1(fc, tgi):
                    slot = fc % 2
                    a_ = a_full[slot]
                    t1_ = t1b[slot][:, tgi * 512:(tgi + 1) * 512]
                    if tgi == 0:
                        cur["g"] = slab_ring.load(wgate_d[l, fc, :, :], 1024)
                        cur["u"] = slab_ring.load(wup_d[l, fc, :, :], 1024)
                    bg, tg_w, sg = cur["g"]
                    bu, tu_w, su = cur["u"]
                    tsl = slice(tgi * 512, (tgi + 1) * 512)
                    b_a = ALLB.get()
                    t_a = mm_group(ps[b_a], [(bg[:, kc * 128:(kc + 1) * 128], h2[:, kc, tsl]) for kc in range(8)], [tg_w, tn[hf]])
                    b_u = ALLB.get()
                    t_u = mm_group(ps[b_u], [(bu[:, kc * 128:(kc + 1) * 128], h2[:, kc, tsl]) for kc in range(8)], [tu_w])
                    if tgi == 1:
                        slab_ring.release(sg, t_u)
                        slab_ring.release(su, t_u)
                    ACT.wait(t_a, t1free[slot][tgi], zb_free)
                    th = None
                    if tgi == 0:
                        ACT.wait(afree[slot], st.get("halo_tok"))
                        afree[slot] = []
                        th = ACT.do(nc.scalar.copy(out=a_[:, 0:2], in_=halo[:, fc, :]))
                    t1free[slot][tgi] = []
                    tc_ = ACT.do(nc.scalar.copy(out=a_[:, 2 + 512 * tgi:514 + 512 * tgi], in_=ps[b_a]))
                    tt1 = ACT.do(nc.scalar.activation(out=t1_, in_=ps[b_a], func=AF.Identity,
                                                      bias=convw[:, l, fc, 3:4], scale=convw[:, l, fc, 2:3]))
                    bank_free[b_a] = [tt1]
                    if tgi == 1 and hf == 0:
                        ACT.wait(tc_)
                        th2 = ACT.do(nc.scalar.copy(out=halo[:, fc, :], in_=a_[:, 1024:1026]))
                        st["halo_tok"] = [th2]
                    DVE.wait(tt1, tc_, th, zb_free)
                    d1 = DVE.do(nc.vector.scalar_tensor_tensor(out=t1_, in0=a_[:, 1 + 512 * tgi:513 + 512 * tgi], scalar=convw[:, l, fc, 1:2],
                                                               in1=t1_, op0=ALU.mult, op1=ALU.add))
                    DVE.wait(d1)
                    d2 = DVE.do(nc.vector.scalar_tensor_tensor(out=t1_, in0=a_[:, 512 * tgi:512 + 512 * tgi], scalar=convw[:, l, fc, 0:1],
                                                               in1=t1_, op0=ALU.mult, op1=ALU.add))
                    return dict(fc=fc, tgi=tgi, slot=slot, t1=t1_, b_u=b_u, t_u=t_u, d2=d2, tsl=tsl)

                def stage2(u):
                    ACT.wait(u["d2"])
                    g1 = ACT.do(nc.scalar.activation(out=u["t1"], in_=u["t1"], func=AF.Gelu_apprx_tanh))
                    DVE.wait(g1, u["t_u"], ut_free)
                    u1 = DVE.do(nc.vector.tensor_tensor(out=uT[:, u["fc"], u["tsl"]], in0=u["t1"], in1=ps[u["b_u"]], op=ALU.mult))
                    bank_free[u["b_u"]] = [u1]
                    t1free[u["slot"]][u["tgi"]] = [u1]
                    if u["tgi"] == 1:
                        afree[u["slot"]] = [u1]
                    return u1

                units = [(fc, tgi) for fc in range(NFC) for tgi in range(2)]
                LAG = 1
                pend = {}
                last_u1 = None
                for i in range(len(units) + LAG):
                    if i < len(units):
                        fc, tgi = units[i]
                        pend[i] = stage1(fc, tgi)
                        if hf == 0 and fc == 6 and tgi == 0:
                            tn[1] = pre_norm(l, 2, h2h[1], [2, 3], F_WDN, nslots=1)
                        if fc == NFC - 3 and tgi == 0:
                            POOL.wait(tn[1] if hf == 0 else st.get("post_free"))
                            for oc in range(2):
                                wl[oc] = wdn_ring.load(wdown_d[l, oc, :, :], DFF)
                    if i >= LAG:
                        last_u1 = stage2(pend.pop(i - LAG))
                zb_free = None
                if hf == 1:
                    DVE.wait(st.get("halo_tok"))
                    tz = DVE.do(nc.vector.memset(halo[:, :, :], 0.0))
                    st["halo_tok"] = [tz]
                zt = []
                tp = None
                for oc in range(8):
                    bw, tw, sw = wl.pop(oc)
                    for tgi in range(2):
                        b = ALLB.get()
                        tp = mm_group(ps[b], [(bw[:, kc * 128:(kc + 1) * 128], uT[:, kc, tgi * 512:(tgi + 1) * 512]) for kc in range(NFC)],
                                      [tw, last_u1])
                        q = evac_eng()
                        q.wait(tp, st.get("post_free"))
                        dst = zA[:, oc, tgi * 512:(tgi + 1) * 512] if oc < 4 else zB[:, oc - 4, tgi * 512:(tgi + 1) * 512]
                        tc_ = copy_on(q, dst, ps[b])
                        bank_free[b] = [tc_]
                        zt.append(tc_)
                    wdn_ring.release(sw, tp)
                    if oc + 2 < 8:
                        wl[oc + 2] = wdn_ring.load(wdown_d[l, oc + 2, :, :], DFF)
                ut_free = [tp]
                for tgi in range(2):
                    tsl = slice(tgi * 512, (tgi + 1) * 512)
                    post_norm_update(l, 3, [(zA[:, :, tsl], 0), (zB[:, :, tsl], 4)], 2 * hf + tgi, zt + [tp], F_WDN, F_WDN + 2048)
                zb_free = st["post_free"]

        for s in range(n_seq):
            load_x(s)
            for l in range(n_layers):
                if do_mixer:
                    mixer(l)
                if do_ffn:
                    ffn(l)
            store_x(s)
        for q in CQ + (SP,):
            q.wait(st["store_tok"])
            q.flush()
    return nc


def _slopes():
    i = np.arange(1, 17, dtype=np.float32)
    return np.exp2(-8.0 * i / 16).astype(np.float32)


def _chunk_layout(w, kdim):
    nk = kdim // 128
    noc = w.shape[1] // 128
    return np.ascontiguousarray(w.reshape(nk, 128, noc, 128).transpose(2, 1, 0, 3).reshape(noc, 128, nk * 128))


def _bf16_hi_lo(a):
    hi = a.astype(ml_dtypes.bfloat16)
    lo = (a - hi.astype(np.float32)).astype(ml_dtypes.bfloat16)
    return hi, lo


def _constants():
    sl = _slopes()
    bf = ml_dtypes.bfloat16
    k = np.arange(128)[:, None].astype(np.float32)
    q = np.arange(256)[None, :].astype(np.float32)
    delta = q - k
    valid = (delta >= 0) & (delta <= 128)
    dmask = np.zeros((128, 12, 256), np.float32)
    for g in range(3):
        for j in range(4):
            h = g * 4 + j
            dmask[:, h, :] = np.where(valid, np.exp(-sl[h] * DILS[g] * np.where(valid, delta, 0.0)), 0.0)
    tri = (np.arange(128)[None, :] >= np.arange(128)[:, None]).astype(np.float32)
    t = np.arange(SEQ)
    qaug = np.zeros((4, 64, SEQ), np.float32)
    kaug = np.zeros((4, 64, SEQ), np.float32)
    qaug_b = np.zeros((4, 64, SEQ), bf)
    kaug_b = np.zeros((4, 64, SEQ), bf)
    for hm in range(4):
        m = np.float32(sl[12 + hm])
        for b in range(8):
            ind = (t // 256 == b).astype(np.float32)
            A = (-m * (t - 256 * b)).astype(np.float32)
            hi, lo = _bf16_hi_lo(A)
            for base in (0, 8, 16):
                kaug_b[hm, base + b] = ind.astype(bf)
            qaug_b[hm, 8 + b] = hi
            qaug_b[hm, 16 + b] = lo
        kp = (m * (t % 256)).astype(np.float32)
        hi, lo = _bf16_hi_lo(kp)
        kaug_b[hm, 24] = hi
        kaug_b[hm, 25] = lo
        qaug_b[hm, 24] = np.ones(SEQ, bf)
        qaug_b[hm, 25] = np.ones(SEQ, bf)
    pastc = np.zeros((128, 3, 16, 8), np.float32)
    for tau in range(16):
        for b in range(8):
            pastc[:, 0, tau, b] = 0.0 if b < tau // 2 else -1e30
            pastc[:, 1, tau, b] = 1.0 if b < tau // 2 else 0.0
            pastc[:, 2, tau, b] = 1.0 if b == tau // 2 else 0.0
    return {
        "dmask": np.ascontiguousarray(dmask.reshape(128, 12 * 256)).astype(bf),
        "tri": tri.astype(bf),
        "qaug_t": qaug_b, "kaug_t": kaug_b,
        "pastc": np.ascontiguousarray(pastc.reshape(128, 384)),
        "ident": np.eye(128, dtype=np.float32),
        "identb": np.eye(128, dtype=np.float32).astype(bf),
    }


def prep_shared(inp, n_layers=2):
    f = lambda a: np.asarray(a, dtype=np.float32)
    sh = {}
    sh["w_in_r"] = np.stack([_chunk_layout(f(inp["w_in"][l]), 1024) for l in range(2)])
    sh["w_brd_r"] = np.stack([_chunk_layout(f(inp["w_branch_dil"][l]), 256) for l in range(2)])
    sh["w_brm_r"] = np.stack([_chunk_layout(f(inp["w_branch_moba"][l]), 256) for l in range(2)])
    sh["w_out_r"] = np.stack([_chunk_layout(f(inp["w_out"][l]), 1024) for l in range(2)])
    sh["w_gate_r"] = np.stack([_chunk_layout(f(inp["w_ffn_gate"][l]), 1024) for l in range(2)])
    sh["w_up_r"] = np.stack([_chunk_layout(f(inp["w_ffn_up"][l]), 1024) for l in range(2)])
    sh["w_down_r"] = np.stack([_chunk_layout(f(inp["w_ffn_down"][l]), DFF) for l in range(2)])
    g = np.stack([f(inp["mix_norm_pre"]), f(inp["mix_norm_post"]), f(inp["ffn_norm_pre"]), f(inp["ffn_norm_post"])], axis=1)
    sh["gains"] = np.ascontiguousarray(g.reshape(2, 4, 8, 128).transpose(3, 0, 1, 2).reshape(128, 64))
    cw = np.concatenate([f(inp["ffn_conv_w"]), f(inp["ffn_conv_b"])[:, None, :]], axis=1)
    sh["convw"] = np.ascontiguousarray(cw.reshape(2, 4, NFC, 128).transpose(3, 0, 2, 1).reshape(128, 2 * NFC * 4))
    sh.update(_constants())
    return sh


_NC_CACHE = {}


def kernel(**inputs):
    x = np.asarray(inputs["x"], dtype=np.float32)
    sh = prep_shared(inputs)
    if "nc" not in _NC_CACHE:
        _NC_CACHE["nc"] = build()
    nc = _NC_CACHE["nc"]
    in_maps = []
    for c in range(NCORES):
        m = dict(sh)
        m["x"] = np.ascontiguousarray(x[2 * c:2 * c + 2])
        in_maps.append(m)
    res = run_bass_kernel_spmd(nc, in_maps, core_ids=list(range(NCORES)))
    out = np.concatenate([np.asarray(r["y"], dtype=np.float32) for r in res.results], axis=0)
    return out
```

```python
import contextlib
import numpy as np
import ml_dtypes
import concourse.bass as bass
import concourse.mybir as mybir
from concourse.bass_utils import run_bass_kernel_spmd

F32 = mybir.dt.float32
BF16 = mybir.dt.bfloat16
AF = mybir.ActivationFunctionType
ALU = mybir.AluOpType
AX = mybir.AxisListType

SEQ = 2048
D = 1024
DFF = 2816
NFC = 22
NCORES = 8
SCALE = 0.125
EPS = 1e-6
DILS = (1, 4, 16)
NEG = -30000.0


class SemObj:
    def __init__(self, nc, es, name):
        self.sem = es.enter_context(nc.semaphore(name))
        self.n = 0


class Q:
    def __init__(self, nc, es, eng, name, attach=False):
        self.eng = eng
        self.so = SemObj(nc, es, "q_" + name)
        self.seen = {}
        self.name = name
        self.attach = attach
        self.pending = None

    def _collect(self, toks, acc):
        for t in toks:
            if t is None:
                continue
            if isinstance(t, (list, tuple)) and not (len(t) == 2 and isinstance(t[0], SemObj)):
                self._collect(t, acc)
                continue
            so, v = t
            if acc.get(so, 0) < v:
                acc[so] = v

    def wait(self, *toks):
        acc = {}
        self._collect(toks, acc)
        for so, v in acc.items():
            if self.seen.get(so, 0) >= v:
                continue
            self.seen[so] = v
            if self.pending is not None:
                self.eng.wait_ge(self.pending[0].sem, self.pending[1])
                self.pending = None
            if self.attach:
                self.pending = (so, v)
            else:
                self.eng.wait_ge(so.sem, v)

    def flush(self):
        if self.pending is not None:
            self.eng.wait_ge(self.pending[0].sem, self.pending[1])
            self.pending = None

    def do(self, inst):
        if self.pending is not None:
            inst._wait_ge(self.pending[0].sem, self.pending[1])
            self.pending = None
        inst.then_inc(self.so.sem, 1)
        self.so.n += 1
        return (self.so, self.so.n)

    def now(self):
        return (self.so, self.so.n) if self.so.n > 0 else None


def build(n_seq=2, n_layers=2, do_mixer=True, do_ffn=True, mixer_stage=99, dump=False):
    nc = bass.Bass("TRN2", target_bir_lowering=False)

    def dt_in(name, shape, dt=F32):
        return nc.dram_tensor(name, list(shape), dt, kind="ExternalInput").ap()

    x_d = dt_in("x", [2, SEQ, D])
    win_d = dt_in("w_in_r", [2, 40, 128, 1024])
    wbrd_d = dt_in("w_brd_r", [2, 8, 128, 256])
    wbrm_d = dt_in("w_brm_r", [2, 8, 128, 256])
    wout_d = dt_in("w_out_r", [2, 8, 128, 1024])
    wgate_d = dt_in("w_gate_r", [2, NFC, 128, 1024])
    wup_d = dt_in("w_up_r", [2, NFC, 128, 1024])
    wdown_d = dt_in("w_down_r", [2, 8, 128, DFF])
    gains_d = dt_in("gains", [128, 2 * 4 * 8])
    convw_d = dt_in("convw", [128, 2 * NFC * 4])
    dmask_d = dt_in("dmask", [128, 12 * 256], BF16)
    tri_d = dt_in("tri", [128, 128], BF16)
    qaug_d = dt_in("qaug_t", [4, 64, SEQ], BF16)
    kaug_d = dt_in("kaug_t", [4, 64, SEQ], BF16)
    pastc_d = dt_in("pastc", [128, 3 * 128])
    ident_d = dt_in("ident", [128, 128])
    identb_d = dt_in("identb", [128, 128], BF16)
    y_d = nc.dram_tensor("y", [2, SEQ, D], F32, kind="ExternalOutput").ap()
    dbg_d = nc.dram_tensor("dbg", [128, 21504 + 8192], F32, kind="ExternalOutput").ap() if dump else None

    es = contextlib.ExitStack()
    with es:
        def sb(name, shape, dt):
            return es.enter_context(nc.sbuf_tensor("s_" + name, list(shape), dt))

        PE = Q(nc, es, nc.tensor, "pe")
        ACT = Q(nc, es, nc.scalar, "act", attach=True)
        DVE = Q(nc, es, nc.vector, "dve", attach=True)
        POOL = Q(nc, es, nc.gpsimd, "pool")
        SP = Q(nc, es, nc.sync, "sp")
        CQ = (PE, ACT, DVE)

        def dma(q, out, in_, so, **kw):
            q.eng.dma_start(out=out, in_=in_, **kw).then_inc(so.sem, 16)
            so.n += 16
            return (so, so.n)

        xT = sb("xT", [128, 8, SEQ], F32)
        hTb = sb("hTb", [128, 8 * SEQ], BF16)
        hT = hTb[:, :].rearrange("p (c t) -> p c t", c=8)
        ident = sb("ident", [128, 128], F32)
        identb = sb("identb", [128, 128], BF16)
        ones_bf = sb("ones_bf", [128, 128], BF16)
        gains = sb("gains", [128, 2, 4, 8], F32)
        convw = sb("convw", [128, 2, NFC, 4], F32)
        dmask = sb("dmask", [128, 12, 256], BF16)
        tri = sb("tri", [128, 128], BF16)
        pastc = sb("pastc", [128, 3, 16, 8], F32)
        epsb = sb("epsb", [128, 1], F32)
        halo = sb("halo", [128, NFC, 2], F32)
        ARENA_W = 21504
        arena = sb("arena", [128, ARENA_W], F32)
        NSLAB = 6
        slabs = sb("slabs", [128, NSLAB, 1024], BF16)
        wbr = sb("wbr", [128, 4, 256], BF16)
        psum = es.enter_context(nc.psum_tensor("psum", [128, 8 * 512], F32))
        ps = [psum[:, 512 * b:512 * (b + 1)] for b in range(8)]
        bank_free = [None] * 8

        def carve(off, nwords, dt=F32):
            assert off + nwords <= ARENA_W, (off, nwords)
            a = arena[:, off:off + nwords]
            if dt == BF16:
                a = a.bitcast(BF16)
            return a

        class BankSet:
            def __init__(self, ids):
                self.ids = list(ids)
                self.i = 0

            def get(self):
                b = self.ids[self.i % len(self.ids)]
                self.i += 1
                PE.wait(bank_free[b])
                bank_free[b] = None
                return b

        ALLB = BankSet(range(8))

        def barrier(extra=()):
            toks = [q.now() for q in CQ] + list(extra)
            for q in CQ:
                q.wait(*toks)
            return toks

        c_so = SemObj(nc, es, "const")
        dma(SP, ident[:, :], ident_d[:, :], c_so)
        dma(SP, identb[:, :], identb_d[:, :], c_so)
        dma(SP, gains[:, :, :, :].rearrange("p a b c -> p (a b c)"), gains_d[:, :], c_so)
        dma(SP, convw[:, :, :, :].rearrange("p a b c -> p (a b c)"), convw_d[:, :], c_so)
        dma(SP, dmask[:, :, :].rearrange("p a b -> p (a b)"), dmask_d[:, :], c_so)
        dma(SP, tri[:, :], tri_d[:, :], c_so)
        CT = dma(SP, pastc[:, :, :, :].rearrange("p a b c -> p (a b c)"), pastc_d[:, :], c_so)
        DVE.do(nc.vector.memset(ones_bf[:, :], 1.0 / 1024.0))
        DVE.do(nc.vector.memset(epsb[:, :], EPS))
        DVE.do(nc.vector.memset(halo[:, :, :], 0.0))
        barrier([CT])

        class Ring:
            def __init__(self, bufs, name):
                self.bufs = bufs
                self.so = [SemObj(nc, es, f"{name}{i}") for i in range(len(bufs))]
                self.free = [[] for _ in bufs]
                self.i = 0

            def load(self, src, n):
                s = self.i % len(self.bufs)
                self.i += 1
                POOL.wait(self.free[s])
                self.free[s] = []
                tok = dma(POOL, self.bufs[s][:, 0:n], src, self.so[s], max_dma_last_dim=4096)
                return self.bufs[s], tok, s

            def release(self, s, tok):
                self.free[s].append(tok)

        slab_ring = Ring([slabs[:, i, :] for i in range(NSLAB)], "slab")
        wbr_ring = Ring([wbr[:, i, :] for i in range(4)], "wbr")
        A_WDN = 11264
        wdn_ring = Ring([carve(A_WDN + i * 1408, 1408, BF16) for i in range(2)], "wdn")

        evac_i = [0]

        def evac_eng():
            evac_i[0] += 1
            return ACT if evac_i[0] % 2 else DVE

        def copy_on(q, out, in_, scale=None):
            if q is ACT:
                if scale is None:
                    return q.do(nc.scalar.copy(out=out, in_=in_))
                return q.do(nc.scalar.activation(out=out, in_=in_, func=AF.Copy, scale=float(scale)))
            if scale is None:
                return q.do(nc.vector.tensor_copy(out=out, in_=in_))
            return q.do(nc.vector.tensor_scalar(out=out, in0=in_, scalar1=float(scale), scalar2=None, op0=ALU.mult))

        def mm_group(out, pairs, waits=()):
            PE.wait(*waits)
            n = len(pairs)
            inst = None
            for i, (l_, r_) in enumerate(pairs):
                inst = nc.tensor.matmul(out, lhsT=l_, rhs=r_, start=(i == 0), stop=(i == n - 1))
            return PE.do(inst)

        NXS = 6
        xst = carve(0, NXS * 1024).rearrange("p (s f) -> p s f", s=NXS)
        xst_so = [SemObj(nc, es, f"xst{i}") for i in range(NXS)]
        yst_so = [SemObj(nc, es, f"yst{i}") for i in range(NXS)]
        st = {}

        def load_x(s):
            barrier([st.get("store_tok")])
            SP.wait([q.now() for q in CQ])
            free = [[] for _ in range(NXS)]
            for tt in range(16):
                sl = tt % NXS
                SP.wait(free[sl])
                free[sl] = []
                tl = dma(SP, xst[:, sl, :], x_d[s, tt * 128:(tt + 1) * 128, :], xst_so[sl])
                tp = None
                for half in range(2):
                    b = ALLB.get()
                    PE.wait(tl)
                    inst = None
                    for j in range(4):
                        c = half * 4 + j
                        inst = nc.tensor.transpose(out=ps[b][:, j * 128:(j + 1) * 128],
                                                   in_=xst[:, sl, c * 128:(c + 1) * 128], identity=ident[:, :])
                    tp = PE.do(inst)
                    q = evac_eng()
                    q.wait(tp)
                    tc_ = copy_on(q, xT[:, half * 4:(half + 1) * 4, tt * 128:(tt + 1) * 128],
                                  ps[b].rearrange("p (c t) -> p c t", c=4))
                    bank_free[b] = [tc_]
                free[sl].append(tp)
            barrier()

        def store_x(s):
            barrier()
            free = [[] for _ in range(NXS)]
            last = None
            for tt in range(16):
                sl = tt % NXS
                tcs = []
                for half in range(2):
                    b = ALLB.get()
                    inst = None
                    for j in range(4):
                        c = half * 4 + j
                        inst = nc.tensor.transpose(out=ps[b][:, j * 128:(j + 1) * 128],
                                                   in_=xT[:, c, tt * 128:(tt + 1) * 128], identity=ident[:, :])
                    tp = PE.do(inst)
                    q = evac_eng()
                    q.wait(tp, free[sl])
                    tc_ = copy_on(q, xst[:, sl, half * 512:(half + 1) * 512], ps[b])
                    bank_free[b] = [tc_]
                    tcs.append(tc_)
                free[sl] = []
                SP.wait(tcs)
                td = dma(SP, y_d[s, tt * 128:(tt + 1) * 128, :], xst[:, sl, :], yst_so[sl])
                free[sl].append(td)
                last = td
            st["store_tok"] = [f for fl in free for f in fl]
            for q in CQ:
                q.wait(st["store_tok"])

        dbg_so = SemObj(nc, es, "dbg")

        def do_dump():
            barrier()
            SP.wait([q.now() for q in CQ])
            dma(SP, dbg_d[:, 0:ARENA_W], arena[:, :], dbg_so)
            t = dma(SP, dbg_d[:, ARENA_W:ARENA_W + 8192], hTb[:, :].bitcast(F32), dbg_so)
            for q in CQ:
                q.wait(t)

        def rstd_from_sq(sq, waits, sd, rstd):
            b = ALLB.get()
            tp = mm_group(ps[b], [(ones_bf[:, :], sq[:, c, :]) for c in range(8)], waits)
            ACT.wait(tp)
            ta = ACT.do(nc.scalar.activation(out=sd, in_=ps[b], func=AF.Sqrt, bias=epsb[:, 0:1], scale=1.0))
            bank_free[b] = [ta]
            DVE.wait(ta)
            tr = DVE.do(nc.vector.reciprocal(out=rstd, in_=sd))
            return tr, tp

        def pre_norm(l, gi, dst, tg_list, scr_off, nslots=2, extra_waits=()):
            sqb = [carve(scr_off + i * 2560, 2048, BF16).rearrange("p (c t) -> p c t", c=8) for i in range(nslots)]
            sdb = [carve(scr_off + i * 2560 + 2048, 512) for i in range(nslots)]
            free = [list(extra_waits) for _ in range(nslots)]
            t = None
            for j, tg in enumerate(tg_list):
                sl = j % nslots
                ACT.wait(free[sl])
                ts = ACT.do(nc.scalar.activation(out=sqb[sl], in_=xT[:, :, tg * 512:(tg + 1) * 512], func=AF.Square))
                DVE.wait(free[sl])
                free[sl] = []
                tr, tp = rstd_from_sq(sqb[sl], [ts], sdb[sl], sdb[sl])
                DVE.wait(tr, extra_waits)
                for c in range(8):
                    t = DVE.do(nc.vector.scalar_tensor_tensor(
                        out=dst[:, c, j * 512:(j + 1) * 512], in0=xT[:, c, tg * 512:(tg + 1) * 512],
                        scalar=gains[:, l, gi, c:c + 1], in1=sdb[sl], op0=ALU.mult, op1=ALU.mult))
                free[sl] = [t, tp]
            return [t, tp]

        def post_norm_update(l, gi, zparts, tg, waits, sq_off, sd_off):
            sqb = carve(sq_off, 2048, BF16).rearrange("p (c t) -> p c t", c=8)
            sd = carve(sd_off, 512)
            fr = st.get("post_free")
            ACT.wait(waits, fr)
            ts = None
            for (zp, c0) in zparts:
                n = zp.shape[1]
                ts = ACT.do(nc.scalar.activation(out=sqb[:, c0:c0 + n, :], in_=zp, func=AF.Square))
            DVE.wait(fr)
            tr, tp = rstd_from_sq(sqb, [ts], sd, sd)
            DVE.wait(tr, waits, ts)
            t1 = None
            for (zp, c0) in zparts:
                n = zp.shape[1]
                t1 = DVE.do(nc.vector.tensor_tensor(out=zp, in0=zp, in1=sd.unsqueeze(1).to_broadcast([128, n, 512]), op=ALU.mult))
            DVE.wait(t1)
            t = None
            for (zp, c0) in zparts:
                for ci in range(zp.shape[1]):
                    c = c0 + ci
                    t = DVE.do(nc.vector.scalar_tensor_tensor(
                        out=xT[:, c, tg * 512:(tg + 1) * 512], in0=zp[:, ci, :], scalar=gains[:, l, gi, c:c + 1],
                        in1=xT[:, c, tg * 512:(tg + 1) * 512], op0=ALU.mult, op1=ALU.add))
            st["post_free"] = [t, tp]
            return t

        A_V = 0
        A_QK = 9216
        A_P = 11264
        A_U = 12800
        A_RD = 16896
        A_OA = 17408
        A_OB = 3072
        A_MISC = 19456
        A_PN = 9216
        A_MT = 6144
        A_Z = 10240
        A_AB = 14336
        A_SQ = 0
        A_SD = 19456

        def mixer(l):
            Vb = [carve(A_V + i * 3072, 3072, BF16).rearrange("p (t c) -> p t c", t=16) for i in range(3)]
            QKb = [carve(A_QK + i * 1024, 1024, BF16) for i in range(2)]
            Pd = [carve(A_P + i * 128, 128, BF16) for i in range(12)]
            Pm = [carve(A_P + i * 256, 256, BF16) for i in range(6)]
            Ub = [carve(A_U + i * 2048, 2048) for i in range(2)]
            rD = carve(A_RD, 512)
            oaT = carve(A_OA, 2048, BF16).rearrange("p (c t) -> p c t", c=2)
            obT = carve(A_OB, 2048, BF16).rearrange("p (c t) -> p c t", c=2)
            m_ones = carve(A_MISC, 32, BF16)
            gm = carve(A_MISC + 64, 128).rearrange("p (t b) -> p t b", b=8)
            mx = carve(A_MISC + 192, 128).rearrange("p (t b) -> p t b", b=8)
            c1 = carve(A_MISC + 320, 128).rearrange("p (t b) -> p t b", b=8)
            selp = carve(A_MISC + 448, 64, BF16).rearrange("p (t b) -> p t b", b=8)
            km = carve(A_MISC + 512, 8)
            kmb = carve(A_MISC + 520, 4, BF16)

            barrier()
            hT_toks = pre_norm(l, 0, hT, [0, 1, 2, 3], A_PN)
            PE.wait(hT_toks)
            t_ones = None
            for i in range(3):
                v6 = Vb[i].rearrange("p t (b c) -> p t b c", c=64)
                t_ones = DVE.do(nc.vector.memset(v6[:, :, 1:5:3, :], 1.0))
            PE.wait(t_ones)

            def load_chunk(ci):
                return slab_ring.load(win_d[l, ci, :, :], 1024)

            def proj_fm(ci, consume):
                buf, tokw, s = load_chunk(ci)
                tp = None
                for tg in range(4):
                    b = ALLB.get()
                    tp = mm_group(ps[b], [(buf[:, kc * 128:(kc + 1) * 128], hT[:, kc, tg * 512:(tg + 1) * 512]) for kc in range(8)], [tokw])
                    tcs = consume(tg, b, tp)
                    bank_free[b] = list(tcs)
                slab_ring.release(s, tp)

            def v_proj(G, V, free_toks):
                c0 = 12 + 2 * G if G < 3 else 22
                b0, t0, s0 = load_chunk(c0)
                b1, t1, s1 = load_chunk(c0 + 1)
                d = DILS[G] if G < 3 else 1
                L = SEQ // d
                toks = []
                tp = None
                for tau in range(16):
                    r = (128 * tau) // L
                    i0 = (128 * tau) % L
                    start = i0 * d + r
                    b = ALLB.get()
                    PE.wait(t0, t1)
                    inst = None
                    assert s1 == s0 + 1
                    for kc in range(8):
                        w0 = b0[:, kc * 128:(kc + 1) * 128]
                        rhs = bass.AP(tensor=w0.tensor, offset=w0.offset, ap=[list(w0.ap[0]), [1024, 2], [1, 128]])
                        inst = nc.tensor.matmul(ps[b][:, 0:256], lhsT=hT[:, kc, start:start + 127 * d + 1:d],
                                                rhs=rhs, start=(kc == 0), stop=(kc == 7))
                    tp = PE.do(inst)
                    q = evac_eng()
                    q.wait(tp, free_toks)
                    v6 = V[:, tau, :].rearrange("p (b c) -> p b c", c=64)
                    p4 = ps[b][:, 0:256].rearrange("p (b c) -> p b c", c=64)
                    q.wait(t_ones)
                    copy_on(q, v6[:, 0:3:2, :], p4[:, 0:2, :])
                    tc_ = copy_on(q, v6[:, 3:6:2, :], p4[:, 2:4, :])
                    bank_free[b] = [tc_]
                    toks.append(tc_)
                slab_ring.release(s0, tp)
                slab_ring.release(s1, tp)
                return toks

            def V_aug(V, tile, hd):
                v = V[:, tile, hd * 64:(hd + 1) * 64]
                return v, m_ones

            SB_ = BankSet([0, 1, 2, 3])
            UBA = BankSet([4, 6])
            UBB = BankSet([5, 7])

            VOFF = (0, 64, 192, 256)

            def pv_mm(out, V, tile, hd, rhs, start, stop):
                return nc.tensor.matmul(out, lhsT=V[:, tile, VOFF[hd]:VOFF[hd] + 128], rhs=rhs, start=start, stop=stop)

            def dil_attention(g, sp, Qs, Ks, qk_toks, V, v_toks, first, acc_free):
                d = DILS[g]
                L = SEQ // d
                nblk = L // 128
                SKEW = 2
                ptoks, pslot = {}, {}
                pfree = st.setdefault("pd_free", [[] for _ in range(12)])
                ucur = {}
                out_toks = []
                lastS = None
                for step in range(16 + SKEW):
                    if step < 16:
                        kt = step
                        nq = 256 if (kt % nblk) < nblk - 1 else 128
                        for h in range(2):
                            hp = 64 * h
                            bS = SB_.get()
                            PE.wait(qk_toks)
                            tS = PE.do(nc.tensor.matmul(ps[bS][:, 0:nq], lhsT=Ks[hp:hp + 64, kt * 128:(kt + 1) * 128],
                                                        rhs=Qs[hp:hp + 64, kt * 128:kt * 128 + nq], start=True, stop=True))
                            lastS = tS
                            sl = st.get("pd_i", 0) % 12
                            st["pd_i"] = st.get("pd_i", 0) + 1
                            ACT.wait(tS, pfree[sl])
                            pfree[sl] = []
                            tE = ACT.do(nc.scalar.activation(out=Pd[sl][:, 0:nq], in_=ps[bS][:, 0:nq], func=AF.Exp))
                            bank_free[bS] = [tE]
                            DVE.wait(tE)
                            head = g * 4 + 2 * sp + h
                            tM = DVE.do(nc.vector.tensor_tensor(out=Pd[sl][:, 0:nq], in0=Pd[sl][:, 0:nq],
                                                                in1=dmask[:, head, 0:nq], op=ALU.mult))
                            ptoks[(h, kt)] = tM
                            pslot[(h, kt)] = sl
                    if step >= SKEW:
                        qb = step - SKEW
                        n = qb % nblk
                        for h in range(2):
                            if qb % 4 == 0:
                                ucur[h] = (UBA if h == 0 else UBB).get()
                            ub = ucur[h]
                            col = (qb % 4) * 128
                            hd = 2 * sp + h
                            lst = []
                            if n > 0:
                                lst.append((qb - 1, pslot[(h, qb - 1)], 128))
                            lst.append((qb, pslot[(h, qb)], 0))
                            PE.wait(v_toks, ptoks[(h, qb)], ptoks.get((h, qb - 1)))
                            inst = None
                            for i, (ktile, sl, pc) in enumerate(lst):
                                inst = pv_mm(ps[ub][:, col:col + 128], V, ktile, hd, Pd[sl][:, pc:pc + 128],
                                             start=(i == 0), stop=(i == len(lst) - 1))
                            tU = PE.do(inst)
                            if n > 0:
                                pfree[pslot[(h, qb - 1)]].append(tU)
                            if n == nblk - 1:
                                pfree[pslot[(h, qb)]].append(tU)
                            if qb % 4 == 3:
                                m = qb // 4
                                U = Ub[h]
                                if d == 1:
                                    dst = U[:, m * 512:(m + 1) * 512]
                                    src = ps[ub]
                                elif d == 4:
                                    dst = U[:, m:SEQ:4]
                                    src = ps[ub]
                                else:
                                    dst = U.rearrange("p (i r) -> p r i", r=16)[:, 4 * m:4 * m + 4, :]
                                    src = ps[ub].rearrange("p (r i) -> p r i", r=4)
                                if first:
                                    ACT.wait(tU, acc_free)
                                    te = copy_on(ACT, dst, src)
                                else:
                                    DVE.wait(tU, st.get("u_last"))
                                    te = DVE.do(nc.vector.tensor_tensor(out=dst, in0=dst, in1=src, op=ALU.add))
                                bank_free[ub] = [te]
                                out_toks.append(te)
                st["u_last"] = out_toks
                return out_toks, lastS

            def normalize_sb(U, u_toks, slot, oT):
                toks = []
                ur = (slot % 2) * 64
                dr = 64 - ur
                for tg in range(4):
                    DVE.wait(u_toks, st.get("rd_free"))
                    t1 = DVE.do(nc.vector.reciprocal(out=rD[ur:ur + 64, :], in_=U[dr:dr + 64, tg * 512:(tg + 1) * 512]))
                    DVE.wait(t1)
                    t2 = DVE.do(nc.vector.tensor_tensor(out=oT[ur:ur + 64, slot // 2, tg * 512:(tg + 1) * 512],
                                                        in0=U[ur:ur + 64, tg * 512:(tg + 1) * 512], in1=rD[ur:ur + 64, :], op=ALU.mult))
                    st["rd_free"] = [t2]
                    toks.append(t2)
                return toks

            vtoks = [v_proj(g, Vb[g], None) for g in range(3)]
            qk_free = None
            acc_free = None
            last_pv = None
            for sp in range(2):
                u_toks = None
                for g in range(3):
                    d = DILS[g]
                    qk_toks = []
                    for which in range(2):
                        ci = which * 6 + g * 2 + sp
                        dstb = QKb[which]

                        def consume(tg, b, tp, dstb=dstb, which=which, d=d):
                            q = evac_eng()
                            q.wait(tp, qk_free)
                            n_i = 512 // d
                            if d == 1:
                                o_ap = dstb[:, tg * 512:(tg + 1) * 512]
                                i_ap = ps[b]
                            else:
                                o_ap = dstb.rearrange("p (r i) -> p r i", r=d)[:, :, tg * n_i:(tg + 1) * n_i]
                                i_ap = ps[b].rearrange("p (i r) -> p r i", r=d)
                            t = copy_on(q, o_ap, i_ap, scale=(SCALE if which == 0 else None))
                            qk_toks.append(t)
                            return [t]
                        proj_fm(ci, consume)
                    u_toks, lastS = dil_attention(g, sp, QKb[0], QKb[1], qk_toks, Vb[g], vtoks[g], g == 0, acc_free)
                    qk_free = [lastS]
                    last_pv = PE.now()
                if sp == 0:
                    nt = []
                    for h in range(2):
                        nt += normalize_sb(Ub[h], u_toks, 2 * sp + h, oaT)
                    acc_free = nt

            def deferred_norm():
                for h in range(2):
                    normalize_sb(Ub[h], u_toks, 2 + h, oaT)

            AUG = [QKb[0], QKb[1], carve(A_V + 6144, 1024, BF16), carve(A_V + 7168, 1024, BF16)]
            vm_toks = v_proj(3, Vb[0], None)
            if "aug_so" not in st:
                st["aug_so"] = [SemObj(nc, es, f"aug{i}") for i in range(4)]
            aug_so = st["aug_so"]
            for hpair in range(2):
                QA, KA, QB, KB = AUG
                heads = (2 * hpair, 2 * hpair + 1)
                SP.wait([q.now() for q in CQ])
                tt_ = [dma(SP, QA[64:128, :], qaug_d[heads[0], :, :], aug_so[0]),
                       dma(SP, KA[64:128, :], kaug_d[heads[0], :, :], aug_so[1]),
                       dma(SP, QB[0:64, :], qaug_d[heads[1], :, :], aug_so[2]),
                       dma(SP, KB[0:64, :], kaug_d[heads[1], :, :], aug_so[3])]
                qk_toks = []
                for which in range(2):
                    ci = 18 + 2 * which + hpair
                    dA, dB = (QA, QB) if which == 0 else (KA, KB)

                    def consume(tg, b, tp, dA=dA, dB=dB, which=which):
                        sc = SCALE if which == 0 else None
                        ACT.wait(tp)
                        ta = copy_on(ACT, dA[0:64, tg * 512:(tg + 1) * 512], ps[b][0:64, :], scale=sc)
                        DVE.wait(tp)
                        tb = copy_on(DVE, dB[64:128, tg * 512:(tg + 1) * 512], ps[b][64:128, :], scale=sc)
                        qk_toks.extend([ta, tb])
                        return [ta, tb]
                    proj_fm(ci, consume)
                if hpair == 0:
                    deferred_norm()
                HX = [(QA, KA, 0, 64), (QB, KB, 64, 0)]
                gmh = [carve(A_MISC + 64 + h * 448, 128).rearrange("p (t b) -> p t b", b=8) for h in range(2)]
                mxh = [carve(A_MISC + 192 + h * 448, 128).rearrange("p (t b) -> p t b", b=8) for h in range(2)]
                c1h = [carve(A_MISC + 320 + h * 448, 128).rearrange("p (t b) -> p t b", b=8) for h in range(2)]
                sph = [carve(A_MISC + 448 + h * 448, 64, BF16).rearrange("p (t b) -> p t b", b=8) for h in range(2)]
                km = carve(A_MISC + 1024, 8)
                kmb = carve(A_MISC + 1032, 4, BF16)
                tk = [None, None]
                for h, (Qx, Kx, r0, e0) in enumerate(HX):
                    DVE.wait(qk_toks, st.get("km_free"))
                    tk[h] = DVE.do(nc.vector.tensor_reduce(out=km[r0:r0 + 64, :], in_=Kx[r0:r0 + 64, :].rearrange("p (b k) -> p b k", b=8),
                                                           axis=AX.X, op=ALU.add))
                for h, (Qx, Kx, r0, e0) in enumerate(HX):
                    DVE.wait(tk[h])
                    tk[h] = DVE.do(nc.vector.tensor_scalar(out=kmb[r0:r0 + 64, :], in0=km[r0:r0 + 64, :], scalar1=1.0 / 256.0, scalar2=None, op0=ALU.mult))
                tgm = [None, None]
                bgs = [None, None]
                for h, (Qx, Kx, r0, e0) in enumerate(HX):
                    bgs[h] = ALLB.get()
                    PE.wait(tk[h], qk_toks)
                    inst = None
                    for tau in range(16):
                        inst = nc.tensor.matmul(ps[bgs[h]][:, tau * 8:(tau + 1) * 8], lhsT=Qx[r0:r0 + 64, tau * 128:(tau + 1) * 128],
                                                rhs=kmb[r0:r0 + 64, :], start=True, stop=True)
                    tgm[h] = PE.do(inst)
                st["km_free"] = list(tgm)
                tc = [None, None]
                for h in range(2):
                    DVE.wait(tgm[h], st.get("gm_free"))
                    tc[h] = DVE.do(nc.vector.tensor_tensor(out=gmh[h], in0=ps[bgs[h]][:, 0:128].rearrange("p (t b) -> p t b", b=8),
                                                           in1=pastc[:, 0, :, :], op=ALU.add))
                    bank_free[bgs[h]] = [tc[h]]
                for h in range(2):
                    DVE.wait(tc[h])
                    for tau in range(16):
                        tc[h] = DVE.do(nc.vector.max(out=mxh[h][:, tau, :], in_=gmh[h][:, tau, :]))
                for h in range(2):
                    DVE.wait(tc[h])
                    tc[h] = DVE.do(nc.vector.tensor_tensor(out=c1h[h], in0=gmh[h], in1=mxh[h][:, :, 2:3].to_broadcast([128, 16, 8]), op=ALU.is_ge))
                for h in range(2):
                    DVE.wait(tc[h])
                    tc[h] = DVE.do(nc.vector.tensor_tensor(out=c1h[h], in0=c1h[h], in1=pastc[:, 1, :, :], op=ALU.mult))
                for h in range(2):
                    DVE.wait(tc[h])
                    tc[h] = DVE.do(nc.vector.tensor_tensor(out=c1h[h], in0=c1h[h], in1=pastc[:, 2, :, :], op=ALU.add))
                for h in range(2):
                    DVE.wait(tc[h])
                    tc[h] = DVE.do(nc.vector.tensor_scalar(out=sph[h], in0=c1h[h], scalar1=-1.0, scalar2=-NEG, op0=ALU.add, op1=ALU.mult))
                tT = [None, None]
                bb12 = [None, None]
                for h in range(2):
                    b1 = ALLB.get()
                    b2 = ALLB.get()
                    bb12[h] = (b1, b2)
                    PE.wait(tc[h])
                    inst = None
                    for tau in range(16):
                        bb = b1 if tau < 8 else b2
                        pso = ps[bb].bitcast(BF16)
                        inst = nc.tensor.transpose(out=pso[0:8, (tau % 8) * 128:(tau % 8 + 1) * 128], in_=sph[h][:, tau, :], identity=identb[:, :])
                    tT[h] = PE.do(inst)
                st["gm_free"] = list(tT)
                for h, (Qx, Kx, r0, e0) in enumerate(HX):
                    b1, b2 = bb12[h]
                    ACT.wait(tT[h], tt_)
                    ta = ACT.do(nc.scalar.copy(out=Qx[e0:e0 + 8, 0:1024], in_=ps[b1].bitcast(BF16)[0:8, :]))
                    DVE.wait(tT[h], tt_)
                    tb = DVE.do(nc.vector.tensor_copy(out=Qx[e0:e0 + 8, 1024:2048], in_=ps[b2].bitcast(BF16)[0:8, :]))
                    bank_free[b1] = [ta]
                    bank_free[b2] = [tb]
                    qk_toks.extend([ta, tb])
                units = [(qg, kt, h) for qg in range(4) for kt in range(4 * qg + 4) for h in range(2)]
                SKEW = 4
                info = {}
                pfree = st.setdefault("pm_free", [[] for _ in range(6)])
                ucur = {}
                for i in range(len(units) + SKEW):
                    if i < len(units):
                        qg, kt, h = units[i]
                        Qx, Kx = (QA, KA) if h == 0 else (QB, KB)
                        c0 = max(0, kt - 4 * qg) * 128
                        nq = 512 - c0
                        bS = SB_.get()
                        PE.wait(qk_toks, tt_)
                        tS = PE.do(nc.tensor.matmul(ps[bS][:, 0:nq], lhsT=Kx[:, kt * 128:(kt + 1) * 128],
                                                    rhs=Qx[:, qg * 512 + c0:(qg + 1) * 512], start=True, stop=True))
                        sl = st.get("pm_i", 0) % 6
                        st["pm_i"] = st.get("pm_i", 0) + 1
                        ACT.wait(tS, pfree[sl])
                        pfree[sl] = []
                        tE = ACT.do(nc.scalar.activation(out=Pm[sl][:, 0:nq], in_=ps[bS][:, 0:nq], func=AF.Exp))
                        bank_free[bS] = [tE]
                        if kt >= 4 * qg:
                            DVE.wait(tE)
                            tE = DVE.do(nc.vector.tensor_tensor(out=Pm[sl][:, 0:128], in0=Pm[sl][:, 0:128], in1=tri[:, :], op=ALU.mult))
                        info[i] = (tE, sl, c0, nq)
                    if i >= SKEW:
                        j = i - SKEW
                        qg, kt, h = units[j]
                        tE, sl, c0, nq = info.pop(j)
                        if kt == 0:
                            ucur[h] = (UBA if h == 0 else UBB).get()
                        ub = ucur[h]
                        hd = 2 * hpair + h
                        PE.wait(tE, vm_toks)
                        last = (kt == 4 * qg + 3)
                        inst = pv_mm(ps[ub][:, c0:512], Vb[0], kt, hd, Pm[sl][:, 0:nq], start=(kt == 0), stop=last)
                        tU = PE.do(inst)
                        pfree[sl].append(tU)
                        if last:
                            DVE.wait(tU, st.get("rd_free"))
                            ur = (hd % 2) * 64
                            dr = 64 - ur
                            t1 = DVE.do(nc.vector.reciprocal(out=rD[ur:ur + 64, :], in_=ps[ub][dr:dr + 64, :]))
                            DVE.wait(t1)
                            t2 = DVE.do(nc.vector.tensor_tensor(out=obT[ur:ur + 64, hd // 2, qg * 512:(qg + 1) * 512],
                                                                in0=ps[ub][ur:ur + 64, :], in1=rD[ur:ur + 64, :], op=ALU.mult))
                            st["rd_free"] = [t2]
                            bank_free[ub] = [t2]
            if dump == "attn":
                do_dump()
                return
            if mixer_stage < 3:
                return

            mT = carve(A_MT, 4096, BF16).rearrange("p (c t) -> p c t", c=8)
            z = carve(A_Z, 4096).rearrange("p (c t) -> p c t", c=8)
            AB = [carve(A_AB + i * 512, 512) for i in range(4)]
            barrier()
            ab_free = [[], [], [], []]
            for hf in range(2):
                mt_toks = []
                for oc in range(8):
                    bga, tga, sga = load_chunk(24 + oc)
                    bgb, tgb, sgb = load_chunk(32 + oc)
                    bwa, twa, swa = wbr_ring.load(wbrd_d[l, oc, :, :], 256)
                    bwm, twm, swm = wbr_ring.load(wbrm_d[l, oc, :, :], 256)
                    tp = None
                    for tgi in range(2):
                        tg = 2 * hf + tgi
                        tsl = slice(tg * 512, (tg + 1) * 512)
                        b_ga = ALLB.get()
                        t_ga = mm_group(ps[b_ga], [(bga[:, kc * 128:(kc + 1) * 128], hT[:, kc, tsl]) for kc in range(8)], [tga])
                        b_gb = ALLB.get()
                        t_gb = mm_group(ps[b_gb], [(bgb[:, kc * 128:(kc + 1) * 128], hT[:, kc, tsl]) for kc in range(8)], [tgb])
                        b_ya = ALLB.get()
                        t_ya = mm_group(ps[b_ya], [(bwa[:, kc * 128:(kc + 1) * 128], oaT[:, kc, tsl]) for kc in range(2)], [twa])
                        b_yb = ALLB.get()
                        t_yb = mm_group(ps[b_yb], [(bwm[:, kc * 128:(kc + 1) * 128], obT[:, kc, tsl]) for kc in range(2)], [twm])
                        tp = t_yb
                        ia = (tgi % 2) * 2
                        A_, B_ = AB[ia], AB[ia + 1]
                        ACT.wait(t_ga, ab_free[ia])
                        ab_free[ia] = []
                        s1 = ACT.do(nc.scalar.activation(out=A_, in_=ps[b_ga], func=AF.Sigmoid))
                        bank_free[b_ga] = [s1]
                        ACT.wait(t_gb, ab_free[ia + 1])
                        ab_free[ia + 1] = []
                        s2 = ACT.do(nc.scalar.activation(out=B_, in_=ps[b_gb], func=AF.Sigmoid))
                        bank_free[b_gb] = [s2]
                        DVE.wait(s1, t_ya)
                        m1 = DVE.do(nc.vector.tensor_tensor(out=A_, in0=A_, in1=ps[b_ya], op=ALU.mult))
                        bank_free[b_ya] = [m1]
                        DVE.wait(s2, t_yb)
                        m2 = DVE.do(nc.vector.tensor_tensor(out=B_, in0=B_, in1=ps[b_yb], op=ALU.mult))
                        bank_free[b_yb] = [m2]
                        DVE.wait(m1, m2, st.get("mt_free"))
                        m3 = DVE.do(nc.vector.tensor_tensor(out=mT[:, oc, tgi * 512:(tgi + 1) * 512], in0=A_, in1=B_, op=ALU.add))
                        ab_free[ia] = [m3]
                        ab_free[ia + 1] = [m3]
                        mt_toks.append(m3)
                    slab_ring.release(sga, tp)
                    slab_ring.release(sgb, tp)
                    wbr_ring.release(swa, tp)
                    wbr_ring.release(swm, tp)
                for tgi in range(2):
                    tg = 2 * hf + tgi
                    zt = []
                    for oc in range(8):
                        bw, tw, sw = slab_ring.load(wout_d[l, oc, :, :], 1024)
                        b = ALLB.get()
                        tp = mm_group(ps[b], [(bw[:, kc * 128:(kc + 1) * 128], mT[:, kc, tgi * 512:(tgi + 1) * 512]) for kc in range(8)], [tw, mt_toks])
                        slab_ring.release(sw, tp)
                        q = evac_eng()
                        q.wait(tp, st.get("post_free"))
                        tc_ = copy_on(q, z[:, oc, :], ps[b])
                        bank_free[b] = [tc_]
                        zt.append(tc_)
                    post_norm_update(l, 1, [(z, 0)], tg, zt, A_SQ, A_SD)
                st["mt_free"] = [PE.now()]
            barrier()

        F_UT = 0
        F_WDN = 11264
        F_A = 14328
        F_T1 = 16384
        F_ZB = 14336

        def ffn(l):
            h2h = [hTb[:, i * 8192:(i + 1) * 8192].rearrange("p (c t) -> p c t", c=8) for i in range(2)]
            zA = hTb[:, 0:8192].bitcast(F32).rearrange("p (c t) -> p c t", c=4)
            zB = carve(F_ZB, 4096).rearrange("p (c t) -> p c t", c=4)
            uT = carve(F_UT, 11264, BF16).rearrange("p (c t) -> p c t", c=NFC)
            a_full = [carve(F_A + i * 1028, 1028) for i in range(2)]
            t1b = [carve(F_T1 + i * 1024, 1024) for i in range(2)]
            barrier()
            st["post_free"] = None
            tn = [pre_norm(l, 2, h2h[0], [0, 1], 0, nslots=2), None]
            afree = [[], []]
            t1free = [[[], []], [[], []]]
            ut_free = None
            zb_free = None
            for hf in range(2):
                h2 = h2h[hf]
                cur = {}
                wl = {}

                def stage1(fc, tgi):
                    slot = fc % 2
                    a_ = a_full[slot]
                    t1_ = t1b[slot][:, tgi * 512:(tgi + 1) * 512]
                    if tgi == 0:
                        cur["g"] = slab_ring.load(wgate_d[l, fc, :, :], 1024)
                        cur["u"] = slab_ring.load(wup_d[l, fc, :, :], 1024)
                    bg, tg_w, sg = cur["g"]
                    bu, tu_w, su = cur["u"]
                    tsl = slice(tgi * 512, (tgi + 1) * 512)
                    b_a = ALLB.get()
                    t_a = mm_group(ps[b_a], [(bg[:, kc * 128:(kc + 1) * 128], h2[:, kc, tsl]) for kc in range(8)], [tg_w, tn[hf]])
                    b_u = ALLB.get()
                    t_u = mm_group(ps[b_u], [(bu[:, kc * 128:(kc + 1) * 128], h2[:, kc, tsl]) for kc in range(8)], [tu_w])
                    if tgi == 1:
                        slab_ring.release(sg, t_u)
                        slab_ring.release(su, t_u)
                    ACT.wait(t_a, t1free[slot][tgi], zb_free)
                    th = None
                    if tgi == 0:
                        ACT.wait(afree[slot], st.get("halo_tok"))
                        afree[slot] = []
                        th = ACT.do(nc.scalar.copy(out=a_[:, 0:2], in_=halo[:, fc, :]))
                    t1free[slot][tgi] = []
                    tc_ = ACT.do(nc.scalar.copy(out=a_[:, 2 + 512 * tgi:514 + 512 * tgi], in_=ps[b_a]))
                    tt1 = ACT.do(nc.scalar.activation(out=t1_, in_=ps[b_a], func=AF.Identity,
                                                      bias=convw[:, l, fc, 3:4], scale=convw[:, l, fc, 2:3]))
                    bank_free[b_a] = [tt1]
                    if tgi == 1 and hf == 0:
                        ACT.wait(tc_)
                        th2 = ACT.do(nc.scalar.copy(out=halo[:, fc, :], in_=a_[:, 1024:1026]))
                        st["halo_tok"] = [th2]
                    DVE.wait(tt1, tc_, th, zb_free)
                    d1 = DVE.do(nc.vector.scalar_tensor_tensor(out=t1_, in0=a_[:, 1 + 512 * tgi:513 + 512 * tgi], scalar=convw[:, l, fc, 1:2],
                                                               in1=t1_, op0=ALU.mult, op1=ALU.add))
                    DVE.wait(d1)
                    d2 = DVE.do(nc.vector.scalar_tensor_tensor(out=t1_, in0=a_[:, 512 * tgi:512 + 512 * tgi], scalar=convw[:, l, fc, 0:1],
                                                               in1=t1_, op0=ALU.mult, op1=ALU.add))
                    return dict(fc=fc, tgi=tgi, slot=slot, t1=t1_, b_u=b_u, t_u=t_u, d2=d2, tsl=tsl)

                def stage2(u):
                    ACT.wait(u["d2"])
                    g1 = ACT.do(nc.scalar.activation(out=u["t1"], in_=u["t1"], func=AF.Gelu_apprx_tanh))
                    DVE.wait(g1, u["t_u"], ut_free)
                    u1 = DVE.do(nc.vector.tensor_tensor(out=uT[:, u["fc"], u["tsl"]], in0=u["t1"], in1=ps[u["b_u"]], op=ALU.mult))
                    bank_free[u["b_u"]] = [u1]
                    t1free[u["slot"]][u["tgi"]] = [u1]
                    if u["tgi"] == 1:
                        afree[u["slot"]] = [u1]
                    return u1

                units = [(fc, tgi) for fc in range(NFC) for tgi in range(2)]
                LAG = 1
                pend = {}
                last_u1 = None
                for i in range(len(units) + LAG):
                    if i < len(units):
                        fc, tgi = units[i]
                        pend[i] = stage1(fc, tgi)
                        if hf == 0 and fc == 6 and tgi == 0:
                            tn[1] = pre_norm(l, 2, h2h[1], [2, 3], F_WDN, nslots=1)
                        if fc == NFC - 3 and tgi == 0:
                            POOL.wait(tn[1] if hf == 0 else st.get("post_free"))
                            for oc in range(2):
                                wl[oc] = wdn_ring.load(wdown_d[l, oc, :, :], DFF)
                    if i >= LAG:
                        last_u1 = stage2(pend.pop(i - LAG))
                zb_free = None
                if hf == 1:
                    DVE.wait(st.get("halo_tok"))
                    tz = DVE.do(nc.vector.memset(halo[:, :, :], 0.0))
                    st["halo_tok"] = [tz]
                zt = []
                tp = None
                for oc in range(8):
                    bw, tw, sw = wl.pop(oc)
                    for tgi in range(2):
                        b = ALLB.get()
                        tp = mm_group(ps[b], [(bw[:, kc * 128:(kc + 1) * 128], uT[:, kc, tgi * 512:(tgi + 1) * 512]) for kc in range(NFC)],
                                      [tw, last_u1])
                        q = evac_eng()
                        q.wait(tp, st.get("post_free"))
                        dst = zA[:, oc, tgi * 512:(tgi + 1) * 512] if oc < 4 else zB[:, oc - 4, tgi * 512:(tgi + 1) * 512]
                        tc_ = copy_on(q, dst, ps[b])
                        bank_free[b] = [tc_]
                        zt.append(tc_)
                    wdn_ring.release(sw, tp)
                    if oc + 2 < 8:
                        wl[oc + 2] = wdn_ring.load(wdown_d[l, oc + 2, :, :], DFF)
                ut_free = [tp]
                for tgi in range(2):
                    tsl = slice(tgi * 512, (tgi + 1) * 512)
                    post_norm_update(l, 3, [(zA[:, :, tsl], 0), (zB[:, :, tsl], 4)], 2 * hf + tgi, zt + [tp], F_WDN, F_WDN + 2048)
                zb_free = st["post_free"]

        for s in range(n_seq):
            load_x(s)
            for l in range(n_layers):
                if do_mixer:
                    mixer(l)
                if do_ffn:
                    ffn(l)
            store_x(s)
        for q in CQ + (SP,):
            q.wait(st["store_tok"])
            q.flush()
    return nc


def _slopes():
    i = np.arange(1, 17, dtype=np.float32)
    return np.exp2(-8.0 * i / 16).astype(np.float32)


def _chunk_layout(w, kdim):
    nk = kdim // 128
    noc = w.shape[1] // 128
    return np.ascontiguousarray(w.reshape(nk, 128, noc, 128).transpose(2, 1, 0, 3).reshape(noc, 128, nk * 128))


def _bf16_hi_lo(a):
    hi = a.astype(ml_dtypes.bfloat16)
    lo = (a - hi.astype(np.float32)).astype(ml_dtypes.bfloat16)
    return hi, lo


def _constants():
    sl = _slopes()
    bf = ml_dtypes.bfloat16
    k = np.arange(128)[:, None].astype(np.float32)
    q = np.arange(256)[None, :].astype(np.float32)
    delta = q - k
    valid = (delta >= 0) & (delta <= 128)
    dmask = np.zeros((128, 12, 256), np.float32)
    for g in range(3):
        for j in range(4):
            h = g * 4 + j
            dmask[:, h, :] = np.where(valid, np.exp(-sl[h] * DILS[g] * np.where(valid, delta, 0.0)), 0.0)
    tri = (np.arange(128)[None, :] >= np.arange(128)[:, None]).astype(np.float32)
    t = np.arange(SEQ)
    qaug = np.zeros((4, 64, SEQ), np.float32)
    kaug = np.zeros((4, 64, SEQ), np.float32)
    qaug_b = np.zeros((4, 64, SEQ), bf)
    kaug_b = np.zeros((4, 64, SEQ), bf)
    for hm in range(4):
        m = np.float32(sl[12 + hm])
        for b in range(8):
            ind = (t // 256 == b).astype(np.float32)
            A = (-m * (t - 256 * b)).astype(np.float32)
            hi, lo = _bf16_hi_lo(A)
            for base in (0, 8, 16):
                kaug_b[hm, base + b] = ind.astype(bf)
            qaug_b[hm, 8 + b] = hi
            qaug_b[hm, 16 + b] = lo
        kp = (m * (t % 256)).astype(np.float32)
        hi, lo = _bf16_hi_lo(kp)
        kaug_b[hm, 24] = hi
        kaug_b[hm, 25] = lo
        qaug_b[hm, 24] = np.ones(SEQ, bf)
        qaug_b[hm, 25] = np.ones(SEQ, bf)
    pastc = np.zeros((128, 3, 16, 8), np.float32)
    for tau in range(16):
        for b in range(8):
            pastc[:, 0, tau, b] = 0.0 if b < tau // 2 else -1e30
            pastc[:, 1, tau, b] = 1.0 if b < tau // 2 else 0.0
            pastc[:, 2, tau, b] = 1.0 if b == tau // 2 else 0.0
    return {
        "dmask": np.ascontiguousarray(dmask.reshape(128, 12 * 256)).astype(bf),
        "tri": tri.astype(bf),
        "qaug_t": qaug_b, "kaug_t": kaug_b,
        "pastc": np.ascontiguousarray(pastc.reshape(128, 384)),
        "ident": np.eye(128, dtype=np.float32),
        "identb": np.eye(128, dtype=np.float32).astype(bf),
    }


def prep_shared(inp, n_layers=2):
    f = lambda a: np.asarray(a, dtype=np.float32)
    sh = {}
    sh["w_in_r"] = np.stack([_chunk_layout(f(inp["w_in"][l]), 1024) for l in range(2)])
    sh["w_brd_r"] = np.stack([_chunk_layout(f(inp["w_branch_dil"][l]), 256) for l in range(2)])
    sh["w_brm_r"] = np.stack([_chunk_layout(f(inp["w_branch_moba"][l]), 256) for l in range(2)])
    sh["w_out_r"] = np.stack([_chunk_layout(f(inp["w_out"][l]), 1024) for l in range(2)])
    sh["w_gate_r"] = np.stack([_chunk_layout(f(inp["w_ffn_gate"][l]), 1024) for l in range(2)])
    sh["w_up_r"] = np.stack([_chunk_layout(f(inp["w_ffn_up"][l]), 1024) for l in range(2)])
    sh["w_down_r"] = np.stack([_chunk_layout(f(inp["w_ffn_down"][l]), DFF) for l in range(2)])
    g = np.stack([f(inp["mix_norm_pre"]), f(inp["mix_norm_post"]), f(inp["ffn_norm_pre"]), f(inp["ffn_norm_post"])], axis=1)
    sh["gains"] = np.ascontiguousarray(g.reshape(2, 4, 8, 128).transpose(3, 0, 1, 2).reshape(128, 64))
    cw = np.concatenate([f(inp["ffn_conv_w"]), f(inp["ffn_conv_b"])[:, None, :]], axis=1)
    sh["convw"] = np.ascontiguousarray(cw.reshape(2, 4, NFC, 128).transpose(3, 0, 2, 1).reshape(128, 2 * NFC * 4))
    sh.update(_constants())
    return sh


_NC_CACHE = {}


def kernel(**inputs):
    x = np.asarray(inputs["x"], dtype=np.float32)
    sh = prep_shared(inputs)
    if "nc" not in _NC_CACHE:
        _NC_CACHE["nc"] = build()
    nc = _NC_CACHE["nc"]
    in_maps = []
    for c in range(NCORES):
        m = dict(sh)
        m["x"] = np.ascontiguousarray(x[2 * c:2 * c + 2])
        in_maps.append(m)
    res = run_bass_kernel_spmd(nc, in_maps, core_ids=list(range(NCORES)))
    out = np.concatenate([np.asarray(r["y"], dtype=np.float32) for r in res.results], axis=0)
    return out
```
